# Optimizing a Trainium2 kernel written in Bass

```python
import jax, jax.numpy as jnp
from jax import lax
import numpy as np

D_MODEL = 1024
BATCH = 4
SEQ = 4096
DEPTH = 1

GRID_W = 64
CTX_LEN = 256
CONV_CH = 512
CONV_GROUPS = 8
LRU_WIDTH = 512
LRU_HEADS = 8
LRU_HEAD_DIM = LRU_WIDTH // LRU_HEADS
MIX_WIDTH = CONV_CH + LRU_WIDTH
IN_COLS = 2 * CONV_CH + 2 * LRU_WIDTH
CONV_TAPS = 31
LRU_CONV_TAPS = 4
LRU_C = 8.0
N_GROUPS = 4
EXPERTS_PER_GROUP = 8
N_EXPERTS = N_GROUPS * EXPERTS_PER_GROUP
TOP_K = 2
D_EXPERT = 1024
MOE_BLOCK = 256
EPS = 1e-6

kernel_name = "hybrid_conformer_rglru_hmoe_prefix_dit"


def rms_norm(x, g):
    xf = x.astype(jnp.float32)
    y = xf * lax.rsqrt(jnp.mean(xf * xf, axis=-1, keepdims=True) + EPS)
    return (y * g.astype(jnp.float32)).astype(x.dtype)


def layer_norm(x, g, b):
    xf = x.astype(jnp.float32)
    mu = jnp.mean(xf, axis=-1, keepdims=True)
    var = jnp.mean(jnp.square(xf - mu), axis=-1, keepdims=True)
    y = (xf - mu) * lax.rsqrt(var + EPS)
    return (y * g.astype(jnp.float32) + b.astype(jnp.float32)).astype(x.dtype)


def modulate(h, shift, scale):
    return h * (1.0 + scale) + shift


def dwconv(u, w, b, pad):
    C = u.shape[-1]
    y = lax.conv_general_dilated(u, w.astype(u.dtype)[:, None, :], window_strides=(1,),
                                 padding=[pad], dimension_numbers=("NWC", "WIO", "NWC"),
                                 feature_group_count=C)
    return y + b.astype(u.dtype)


def conformer_conv(val, gate, dw, b, ln_g, ln_b):
    u = val * jax.nn.sigmoid(gate)
    u = dwconv(u, dw, b, (CONV_TAPS // 2, CONV_TAPS // 2))
    return jax.nn.silu(layer_norm(u, ln_g, ln_b))


def rglru_coeffs(u, wa, ba, wx, bx, lam):
    B_, L, C = u.shape
    uf = u.astype(jnp.float32)
    uh = uf.reshape(B_, L, LRU_HEADS, LRU_HEAD_DIM)
    r = jax.nn.sigmoid(jnp.einsum("blhi,hij->blhj", uh, wa.astype(jnp.float32)).reshape(B_, L, C) + ba.astype(jnp.float32))
    i = jax.nn.sigmoid(jnp.einsum("blhi,hij->blhj", uh, wx.astype(jnp.float32)).reshape(B_, L, C) + bx.astype(jnp.float32))
    log_a = -LRU_C * r * jax.nn.softplus(-lam.astype(jnp.float32))
    a = jnp.exp(log_a)
    mult = jnp.sqrt(jnp.maximum(-jnp.expm1(2.0 * log_a), 1e-12))
    return a, mult * i * uf


def linear_scan(a, b, h0):
    b = b.at[:, 0].add(a[:, 0] * h0)

    def combine(left, right):
        return (left[0] * right[0], right[0] * left[1] + right[1])

    _, h = lax.associative_scan(combine, (a, b), axis=1)
    return h


def directional_scan(a, b, h0, reverse):
    if reverse:
        return jnp.flip(linear_scan(jnp.flip(a, 1), jnp.flip(b, 1), h0), 1)
    return linear_scan(a, b, h0)


def swiglu_expert(xb, w1, w3, w2):
    return (jax.nn.silu(xb @ w1) * (xb @ w3)) @ w2


def hier_moe(h, wg, bg, we, be, w1, w3, w2):
    T, D = h.shape
    hf = h.astype(jnp.float32)
    pg = jax.nn.softmax(hf @ wg.astype(jnp.float32) + bg.astype(jnp.float32), axis=-1)
    p_grp, grp = lax.top_k(pg, 1)
    p_grp, grp = p_grp[:, 0], grp[:, 0]
    le = jnp.einsum("td,dge->tge", hf, we.astype(jnp.float32)) + be.astype(jnp.float32)
    le = jnp.take_along_axis(le, grp[:, None, None], axis=1)[:, 0]
    top_p, top_i = lax.top_k(jax.nn.softmax(le, axis=-1), TOP_K)
    gates = p_grp[:, None] * top_p / jnp.sum(top_p, axis=-1, keepdims=True)
    eid = (grp[:, None] * EXPERTS_PER_GROUP + top_i).reshape(-1)
    tok = jnp.repeat(jnp.arange(T, dtype=jnp.int32), TOP_K)
    gflat = gates.reshape(-1)
    order = jnp.argsort(eid)
    e_sorted, tok_sorted, g_sorted = eid[order], tok[order], gflat[order]
    counts = jnp.bincount(eid, length=N_EXPERTS)
    padded = ((counts + MOE_BLOCK - 1) // MOE_BLOCK) * MOE_BLOCK
    start = jnp.cumsum(counts) - counts
    pend = jnp.cumsum(padded)
    pstart = pend - padded
    dest = pstart[e_sorted] + (jnp.arange(T * TOP_K) - start[e_sorted])
    n_rows = -(-(T * TOP_K) // MOE_BLOCK) * MOE_BLOCK + N_EXPERTS * MOE_BLOCK
    n_blocks = n_rows // MOE_BLOCK
    tok_buf = jnp.zeros((n_rows,), jnp.int32).at[dest].set(tok_sorted)
    gate_buf = jnp.zeros((n_rows,), h.dtype).at[dest].set(g_sorted.astype(h.dtype))
    blk_e = jnp.minimum(jnp.searchsorted(pend, jnp.arange(n_blocks) * MOE_BLOCK, side="right"), N_EXPERTS - 1)
    xb = h[tok_buf].reshape(n_blocks, MOE_BLOCK, D)

    def run(args):
        xblk, e = args
        return swiglu_expert(xblk, w1[e], w3[e], w2[e])

    yb = lax.map(run, (xb, blk_e)).reshape(n_rows, D)
    return jnp.zeros_like(h).at[tok_buf].add(yb * gate_buf[:, None])


def setup_inputs(seed: int = 0) -> dict:
    key = jax.random.key(seed)
    ks = jax.random.split(key, 32)
    f32 = jnp.float32

    def n(k, shape, s):
        return jax.random.normal(k, shape, f32) * s

    L = DEPTH
    u = jax.random.uniform(ks[20], (L, 2, LRU_WIDTH), f32, 0.9, 0.999)
    a0 = u ** (1.0 / LRU_C)
    lam = jnp.log(a0) - jnp.log1p(-a0)
    return {
        "x": n(ks[0], (BATCH, SEQ, D_MODEL), 1.0),
        "c": n(ks[1], (BATCH, D_MODEL), 1.0),
        "ctx": n(ks[2], (BATCH, CTX_LEN, D_MODEL), 1.0),
        "c_ctx": n(ks[3], (D_MODEL,), 1.0),
        "w_ada": n(ks[4], (L, D_MODEL, 6 * D_MODEL), 0.5 * D_MODEL ** -0.5),
        "b_ada": n(ks[5], (L, 6 * D_MODEL), 0.02),
        "norm1_g": 1.0 + n(ks[6], (L, D_MODEL), 0.02),
        "norm2_g": 1.0 + n(ks[7], (L, D_MODEL), 0.02),
        "w_in": n(ks[8], (L, D_MODEL, IN_COLS), D_MODEL ** -0.5),
        "conv_dw": n(ks[9], (L, CONV_TAPS, CONV_CH), CONV_TAPS ** -0.5),
        "conv_b": n(ks[10], (L, CONV_CH), 0.02),
        "conv_ln_g": 1.0 + n(ks[11], (L, CONV_CH), 0.02),
        "conv_ln_b": n(ks[12], (L, CONV_CH), 0.02),
        "lru_conv_w": n(ks[13], (L, LRU_CONV_TAPS, LRU_WIDTH), LRU_CONV_TAPS ** -0.5),
        "lru_conv_b": n(ks[14], (L, LRU_WIDTH), 0.02),
        "lru_wa": n(ks[15], (L, 2, LRU_HEADS, LRU_HEAD_DIM, LRU_HEAD_DIM), LRU_HEAD_DIM ** -0.5),
        "lru_ba": n(ks[16], (L, 2, LRU_WIDTH), 0.02),
        "lru_wx": n(ks[17], (L, 2, LRU_HEADS, LRU_HEAD_DIM, LRU_HEAD_DIM), LRU_HEAD_DIM ** -0.5),
        "lru_bx": n(ks[18], (L, 2, LRU_WIDTH), 0.02),
        "lru_lam": lam,
        "w_out": n(ks[19], (L, MIX_WIDTH, D_MODEL), MIX_WIDTH ** -0.5),
        "router_wg": n(ks[21], (L, D_MODEL, N_GROUPS), D_MODEL ** -0.5),
        "router_bg": n(ks[22], (L, N_GROUPS), 0.01),
        "router_we": n(ks[23], (L, D_MODEL, N_GROUPS, EXPERTS_PER_GROUP), D_MODEL ** -0.5),
        "router_be": n(ks[24], (L, N_GROUPS, EXPERTS_PER_GROUP), 0.01),
        "w1": n(ks[25], (L, N_EXPERTS, D_MODEL, D_EXPERT), D_MODEL ** -0.5),
        "w3": n(ks[26], (L, N_EXPERTS, D_MODEL, D_EXPERT), D_MODEL ** -0.5),
        "w2": n(ks[27], (L, N_EXPERTS, D_EXPERT, D_MODEL), D_EXPERT ** -0.5),
        "final_g": 1.0 + n(ks[28], (D_MODEL,), 0.02),
    }


def reference(x, c, ctx, c_ctx, w_ada, b_ada, norm1_g, norm2_g, w_in, conv_dw, conv_b, conv_ln_g, conv_ln_b,
              lru_conv_w, lru_conv_b, lru_wa, lru_ba, lru_wx, lru_bx, lru_lam, w_out,
              router_wg, router_bg, router_we, router_be, w1, w3, w2, final_g):
    B, S, D = x.shape
    rows = S // GRID_W
    xc = ctx
    Cc, Lw = CONV_CH, LRU_WIDTH
    for l in range(DEPTH):
        last = l == DEPTH - 1
        mod_l = (jax.nn.silu(c) @ w_ada[l] + b_ada[l])[:, None, :]
        mod_c = jax.nn.silu(c_ctx) @ w_ada[l] + b_ada[l]
        sh1, sc1, g1, sh2, sc2, g2 = jnp.split(mod_l, 6, axis=-1)
        csh1, csc1, cg1, csh2, csc2, cg2 = jnp.split(mod_c, 6, axis=-1)

        zl = modulate(rms_norm(x, norm1_g[l]), sh1, sc1) @ w_in[l]
        zc = modulate(rms_norm(xc, norm1_g[l]), csh1, csc1) @ w_in[l]

        conv_l = conformer_conv(zl[..., :Cc].reshape(B * rows, GRID_W, Cc),
                                zl[..., Cc:2 * Cc].reshape(B * rows, GRID_W, Cc),
                                conv_dw[l], conv_b[l], conv_ln_g[l], conv_ln_b[l]).reshape(B, S, Cc)

        ul = dwconv(zl[..., 2 * Cc:2 * Cc + Lw], lru_conv_w[l], lru_conv_b[l], (2, 1))
        uc = dwconv(zc[..., 2 * Cc:2 * Cc + Lw], lru_conv_w[l], lru_conv_b[l], (2, 1))
        h_lat = jnp.zeros((B, S, Lw), jnp.float32)
        h_ctx = jnp.zeros((B, uc.shape[1], Lw), jnp.float32)
        for d, reverse in enumerate((False, True)):
            ac, bc = rglru_coeffs(uc, lru_wa[l, d], lru_ba[l, d], lru_wx[l, d], lru_bx[l, d], lru_lam[l, d])
            al, bl = rglru_coeffs(ul, lru_wa[l, d], lru_ba[l, d], lru_wx[l, d], lru_bx[l, d], lru_lam[l, d])
            hc_d = directional_scan(ac, bc, jnp.zeros((B, Lw), jnp.float32), reverse)
            h0 = hc_d[:, 0] if reverse else hc_d[:, -1]
            h_lat = h_lat + directional_scan(al, bl, h0, reverse)
            if not last:
                h_ctx = h_ctx + hc_d
        lru_l = h_lat.astype(x.dtype) * jax.nn.gelu(zl[..., 2 * Cc + Lw:])

        x = x + g1 * (jnp.concatenate([conv_l, lru_l], axis=-1) @ w_out[l])

        if not last:
            conv_c = conformer_conv(zc[..., :Cc], zc[..., Cc:2 * Cc],
                                    conv_dw[l], conv_b[l], conv_ln_g[l], conv_ln_b[l])
            lru_c = h_ctx.astype(xc.dtype) * jax.nn.gelu(zc[..., 2 * Cc + Lw:])
            xc = xc + cg1 * (jnp.concatenate([conv_c, lru_c], axis=-1) @ w_out[l])

        h2 = modulate(rms_norm(x, norm2_g[l]), sh2, sc2).reshape(B * S, D)
        y2 = hier_moe(h2, router_wg[l], router_bg[l], router_we[l], router_be[l], w1[l], w3[l], w2[l])
        x = x + g2 * y2.reshape(B, S, D)

        if not last:
            hc2 = modulate(rms_norm(xc, norm2_g[l]), csh2, csc2).reshape(-1, D)
            yc2 = hier_moe(hc2, router_wg[l], router_bg[l], router_we[l], router_be[l], w1[l], w3[l], w2[l])
            xc = xc + cg2 * yc2.reshape(xc.shape)

    return rms_norm(x, final_g)
```

```python
import numpy as np
from contextlib import ExitStack
import concourse.bass as bass
import concourse.mybir as mybir
from concourse.bass_utils import run_bass_kernel_spmd

F32 = mybir.dt.float32
BF16 = mybir.dt.bfloat16
I32 = mybir.dt.int32
AF = mybir.ActivationFunctionType
ALU = mybir.AluOpType
AX = mybir.AxisListType

BS = 256
NSLOT = 48
NR = NSLOT * BS
NV = 68
NCV = 14


class Sched:
    ENG = ("pe", "act", "dve", "pool", "sp")

    def __init__(self, nc, stack):
        self.nc = nc
        self.stack = stack
        self.ops = {e: [] for e in self.ENG}
        self.cnt = {e: 0 for e in self.ENG}
        self.known = {e: {} for e in self.ENG}
        self.sem = {}
        self.reg = {}
        self.dstate = {}
        for e in self.ENG:
            self._sem("E:" + e)

    def _sem(self, key):
        if key not in self.sem:
            self.sem[key] = self.stack.enter_context(self.nc.semaphore(key.replace(":", "_")))
        return self.sem[key]

    def _deps(self, eng, reads, writes):
        deps = {}

        def add(k, v):
            if deps.get(k, 0) < v:
                deps[k] = v
        for k in reads:
            r = self.reg.get(k)
            if r and r[0]:
                add(*r[0])
        for k in writes:
            r = self.reg.get(k)
            if r:
                if r[0]:
                    add(*r[0])
                for sk, v in r[1].items():
                    add(sk, v)
        waits = []
        kn = self.known[eng]
        for sk, v in deps.items():
            if sk == "E:pe" and eng == "pe":
                continue
            if kn.get(sk, 0) < v:
                kn[sk] = v
                waits.append((sk, v))
        return waits

    def _mark(self, tok, reads, writes):
        for k in reads:
            r = self.reg.setdefault(k, [None, {}])
            if r[1].get(tok[0], 0) < tok[1]:
                r[1][tok[0]] = tok[1]
        for k in writes:
            self.reg[k] = [tok, {}]

    def op(self, eng, fn, reads=(), writes=(), inc=True):
        reads = list(reads)
        writes = list(writes)
        waits = self._deps(eng, reads, writes)
        sk = "E:" + eng
        if inc:
            self.cnt[eng] += 1
            tok = (sk, self.cnt[eng])
        else:
            tok = (sk, self.cnt[eng] + 1)
        self.ops[eng].append((waits, fn, (sk, 1) if inc else None))
        self._mark(tok, reads, writes)

    def dma(self, q, fn, reads=(), writes=(), sem="d", last=True):
        reads = list(reads)
        writes = list(writes)
        sk = "D:" + sem
        self._sem(sk)
        st = self.dstate.setdefault(sk, {"tot": 0, "open": []})
        waits = self._deps(q, reads, writes)
        if not st["open"] and st["tot"] > 0 and self.known[q].get(sk, 0) < st["tot"]:
            waits.append((sk, st["tot"]))
            self.known[q][sk] = st["tot"]
        st["tot"] += 16
        st["open"].append((reads, writes))
        self.ops[q].append((waits, fn, (sk, 16)))
        if last:
            tok = (sk, st["tot"])
            for r, w in st["open"]:
                self._mark(tok, r, w)
            st["open"] = []

    def barrier(self):
        for x in self.ENG:
            waits = []
            for e in self.ENG:
                if e == x:
                    continue
                sk = "E:" + e
                if self.cnt[e] > self.known[x].get(sk, 0):
                    self.known[x][sk] = self.cnt[e]
                    waits.append((sk, self.cnt[e]))
            for sk, st in self.dstate.items():
                assert not st["open"]
                if st["tot"] > self.known[x].get(sk, 0):
                    self.known[x][sk] = st["tot"]
                    waits.append((sk, st["tot"]))
            self.ops[x].append((waits, None, None))

    def final_wait(self, q, keys):
        waits = self._deps(q, list(keys), [])
        self.ops[q].append((waits, None, None))

    def emit(self):
        sems = self.sem
        with self.nc.Block() as block:
            def mk(name):
                def body(e):
                    for waits, fn, inc in self.ops[name]:
                        for sk, v in waits:
                            e.wait_ge(sems[sk], v)
                        if fn is None:
                            continue
                        ins = fn(e)
                        if inc is not None:
                            ins.then_inc(sems[inc[0]], inc[1])
                return body
            block.tensor(mk("pe"))
            block.scalar(mk("act"))
            block.vector(mk("dve"))
            block.gpsimd(mk("pool"))
            block.sync(mk("sp"))


class Arena:
    def __init__(self, base, nbytes):
        self.base = base
        self.total = nbytes
        self.off = 0

    def alloc(self, shape, dt):
        n = int(np.prod(shape))
        sz = n * (4 if dt in (F32, I32) else 2)
        szp = (sz + 31) // 32 * 32
        off = self.off
        self.off += szp
        assert self.off <= self.total, ("SBUF arena overflow", self.off, self.total)
        v = self.base[:, off // 4:(off + szp) // 4]
        if dt != F32:
            v = v.bitcast(dt)
        v = v[:, 0:n]
        if len(shape) == 2:
            v = v.rearrange("p (a b) -> p a b", a=shape[0])
        elif len(shape) == 3:
            v = v.rearrange("p (a b c) -> p a b c", a=shape[0], b=shape[1])
        return v


def build_nc(stage=9):
    nc = bass.Bass("TRN2", target_bir_lowering=False)

    def D(name, shape, dt, kind="ExternalInput"):
        return nc.dram_tensor(name, shape, dt, kind=kind).ap()
    xT = D("xT", [1024, 4096], F32)
    xtok = D("xtok", [2048, 1024], F32)
    ctxT = D("ctxT", [1024, 256], F32)
    cvec = D("cvec", [1024, 2], F32)
    w_ada = D("w_ada", [1024, 6144], F32)
    b_ada = D("b_ada", [128, 96], F32)
    vecs_d = D("vecs", [128, NV], F32)
    n2g_d = D("n2g", [128, 1024], F32)
    fg_d = D("fg", [128, 1024], F32)
    w_in = D("w_in", [1024, 2048], F32)
    w_out = D("w_out", [1024, 1024], F32)
    cdw_d = D("cdw", [128, 124], F32)
    lbd_d = D("lbd", [128, 2048], F32)
    wr_d = D("wr", [1024, 36], F32)
    br_d = D("br", [128, 36], F32)
    w1 = D("w1", [32768, 1024], F32)
    w3 = D("w3", [32768, 1024], F32)
    w2 = D("w2", [32768, 1024], F32)
    out = D("out", [2048, 1024], F32, "ExternalOutput")
    x1_d = D("x1_d", [2048, 1024], F32, "Internal")
    h2_d = D("h2_d", [2048, 1024], BF16, "Internal")
    xs_d = D("xs_d", [NR, 1024], BF16, "Internal")
    y_d = D("y_d", [NR, 1024], F32, "Internal")
    wbf = [D("wbf%d" % m, [NCV * 1024, 1024], BF16, "Internal") for m in range(3)]

    with ExitStack() as st:
        S = Sched(nc, st)
        ARB = 211968
        arena_t = st.enter_context(nc.sbuf_tensor("arena", [128, ARB // 4], F32))
        AR = Arena(arena_t[:, :], ARB)
        PSB = [st.enter_context(nc.psum_tensor("psb%d" % i, [128, 512], F32))[:, :] for i in range(8)]

        def PK(i):
            return ("ps", i)

        def mm(out_, lhsT, rhs, start, stop, reads, writes, inc=None):
            if inc is None:
                inc = stop
            S.op("pe", lambda e: e.matmul(out_, lhsT, rhs, start=start, stop=stop), reads, writes, inc=inc)

        def act(out_, in_, func, reads, writes, bias=None, scale=None, accum=None):
            kw = {}
            if bias is not None:
                kw["bias"] = bias
            if scale is not None:
                kw["scale"] = scale
            if accum is not None:
                kw["accum_out"] = accum
            S.op("act", lambda e: e.activation(out=out_, in_=in_, func=func, **kw), reads, writes)

        def tt(eng, out_, a, b, op, reads, writes):
            S.op(eng, lambda e: e.tensor_tensor(out=out_, in0=a, in1=b, op=op), reads, writes)

        def ts(eng, out_, a, s1, s2, op0, op1, reads, writes):
            if s2 is None:
                S.op(eng, lambda e: e.tensor_single_scalar(out=out_, in_=a, scalar=s1, op=op0), reads, writes)
            else:
                S.op(eng, lambda e: e.tensor_scalar(out=out_, in0=a, scalar1=s1, scalar2=s2, op0=op0, op1=op1), reads, writes)

        def stt(eng, out_, a, sc, b, op0, op1, reads, writes):
            S.op(eng, lambda e: e.scalar_tensor_tensor(out=out_, in0=a, scalar=sc, in1=b, op0=op0, op1=op1), reads, writes)

        def cp(eng, out_, in_, reads, writes):
            if eng == "act":
                S.op("act", lambda e: e.copy(out=out_, in_=in_), reads, writes)
            else:
                S.op(eng, lambda e: e.tensor_copy(out=out_, in_=in_), reads, writes)

        def recip(out_, in_, reads, writes):
            S.op("dve", lambda e: e.reciprocal(out=out_, in_=in_), reads, writes)

        def mset(eng, ap, val, writes):
            S.op(eng, lambda e: e.memset(ap, val), [], writes)

        def dma(q, out_, in_, reads, writes, sem, last=True):
            S.dma(q, lambda e: e.dma_start(out=out_, in_=in_), reads, writes, sem=sem, last=last)

        def red(eng, out_, in_, op, reads, writes):
            S.op(eng, lambda e: e.tensor_reduce(out=out_, in_=in_, axis=AX.X, op=op), reads, writes)

        wsrc = [w1, w3, w2]
        cv_list = [(m, e) for e in range(32 - NCV, 32) for m in range(3)]
        cv_pos = [0]

        def cv_next(n=1):
            for _ in range(n):
                if cv_pos[0] >= len(cv_list):
                    return
                m, e = cv_list[cv_pos[0]]
                i = cv_pos[0]
                cv_pos[0] += 1
                e0 = e - (32 - NCV)
                src = wsrc[m][e * 1024:(e + 1) * 1024, :]
                dst = wbf[m][e0 * 1024:(e0 + 1) * 1024, :]
                dma("pool", dst, src, [], [("wbf", m, e)], "cv%d" % (i % 4))

        iota_f = AR.alloc([128], F32)
        ident_f = AR.alloc([128], F32)
        ones_f = AR.alloc([128], F32)
        ident_b = AR.alloc([128], BF16)
        ones_b = AR.alloc([128], BF16)
        ones512_b = AR.alloc([128], BF16)
        U_b = AR.alloc([128], BF16)
        eps_t = AR.alloc([1], F32)
        one_t = AR.alloc([1], F32)
        vecs = AR.alloc([NV], F32)
        cdw = AR.alloc([124], F32)
        mod_fm = AR.alloc([96], F32)
        gs1 = AR.alloc([8, 2], F32)
        zbias = AR.alloc([32], F32)
        cs = AR.alloc([8], F32)
        hcs = AR.alloc([8], F32)
        hb = AR.alloc([24], F32)
        cv = AR.alloc([8, 2], F32)
        scT = AR.alloc([8, 2], BF16)
        scTf = AR.alloc([8, 2], F32)
        bada = AR.alloc([96], F32)
        carry = AR.alloc([8], F32)
        ldiag = AR.alloc([20, 128], BF16)
        lbd = AR.alloc([16, 128], BF16)
        lg_all = AR.alloc([16, 36], F32)
        PERSIST = AR.off
        MOEBASE = PERSIST

        V_N1G, V_CB, V_LNG, V_LNB, V_LCB = 0, 8, 12, 16, 20
        V_BA = (24, 36)
        V_BX = (28, 40)
        V_LAM = (32, 44)
        V_W5 = 48

        def vcol(i):
            return vecs[:, i:i + 1]

        def modv(v, t, r):
            c = (v * 8 + t) * 2 + r
            return mod_fm[:, c:c + 1]

        S.op("pool", lambda e: e.iota(iota_f, pattern=[[1, 128]], base=0, channel_multiplier=-1,
                                      allow_small_or_imprecise_dtypes=True), [], ["iota"])
        ts("dve", ident_f, iota_f, 0.0, None, ALU.is_equal, None, ["iota"], ["ident_f"])
        ts("dve", ident_b, iota_f, 0.0, None, ALU.is_equal, None, ["iota"], ["ident_b"])
        ts("dve", U_b, iota_f, 0.0, None, ALU.is_gt, None, ["iota"], ["U_b"])
        mset("pool", ones_f, 1.0, ["ones_f"])
        mset("pool", ones_b, 1.0, ["ones_b"])
        mset("pool", ones512_b, 1.0 / 512.0, ["ones512_b"])
        mset("pool", eps_t, 1e-6, ["eps"])
        mset("pool", one_t, 1.0, ["one"])
        dma("sp", vecs, vecs_d, [], ["vecs"], "c0")
        dma("sp", cdw, cdw_d, [], ["cdw"], "c1")
        dma("sp", cv, cvec.rearrange("(t p) r -> p t r", p=128), [], ["cv"], "c6")
        dma("sp", bada, b_ada, [], ["bada"], "c7")
        dma("pool", lbd.rearrange("p a b -> p (a b)"), lbd_d, [], ["lbd"], "c5")
        act(scT, cv, AF.Silu, ["cv"], ["scT"])
        act(scTf, cv, AF.Silu, ["cv"], ["scTf"])

        zl_buf = AR.alloc([4, 4100], BF16)
        zc_buf = AR.alloc([4, 260], BF16)
        convT = AR.alloc([4, 2048], BF16)
        gel = AR.alloc([4, 2048], BF16)
        markM = AR.off
        w_in_b = AR.alloc([8, 2048], BF16)
        sh1_b = AR.alloc([8, 2], BF16)
        cdg = [AR.alloc([31, 128], BF16) for _ in range(2)]
        xc = AR.alloc([8, 512], F32)
        sqhm = AR.alloc([8, 512], BF16)
        sqb = AR.alloc([8, 512], BF16)
        rtb = [AR.alloc([512], F32) for _ in range(2)]
        upad = AR.alloc([4, 8, 94], BF16)
        cc = AR.alloc([4, 512], F32)
        cbf = AR.alloc([4, 512], BF16)
        csq = AR.alloc([4, 512], BF16)
        ztile = AR.alloc([512], BF16)
        NT = 9
        tmp = [AR.alloc([512], F32) for _ in range(NT)]
        tctr = [0]
        print("M1 arena bytes", AR.off)

        def T():
            i = tctr[0] % NT
            tctr[0] += 1
            return tmp[i], ("tmp", i)

        psM = PSB[7]
        wab = [xc[:, :, 0:256].bitcast(BF16), sqhm]
        wabk = [["xc"], [("sqhm", t) for t in range(8)]]

        def ada_part(c_lo, c_hi, col0):
            for c in range(c_lo, c_hi):
                b = c % 2
                dma("pool", wab[b], w_ada.rearrange("(t p) n -> p t n", p=128)[:, :, c * 512:(c + 1) * 512],
                    [], wabk[b], "wab%d" % b)
                for ctl in range(4):
                    ct = c * 4 + ctl
                    for kt in range(8):
                        mm(psM[:, (ct - col0) * 2:(ct - col0) * 2 + 2], wab[b][:, kt, ctl * 128:(ctl + 1) * 128],
                           scT[:, kt, :], kt == 0, kt == 7, wabk[b] + ["scT"], [PK(7)])

        ada_part(0, 4, 0)
        tt("dve", mod_fm[:, 0:32], psM[:, 0:32], bada[:, 0:32], ALU.add, [PK(7), "bada"], ["mod_a"])
        sc1v = mod_fm[:, 16:32].rearrange("p (t r) -> p t r", r=2)
        sh1v = mod_fm[:, 0:16].rearrange("p (t r) -> p t r", r=2)
        for r in range(2):
            stt("dve", gs1[:, :, r], sc1v[:, :, r], 1.0, vecs[:, V_N1G:V_N1G + 8], ALU.add, ALU.mult,
                ["mod_a", "vecs"], ["gs1"])
        cp("dve", sh1_b, sh1v, ["mod_a"], ["sh1_b"])
        for d in range(2):
            lamv = vecs[:, V_LAM[d]:V_LAM[d] + 4]
            act(cs[:, d * 4:d * 4 + 4], lamv, AF.Exp, ["vecs"], [("cs", d)], scale=-1.0)
            act(cs[:, d * 4:d * 4 + 4], cs[:, d * 4:d * 4 + 4], AF.Ln, [("cs", d), "one"], [("cs", d)], bias=one_t)
            ts("dve", hcs[:, d * 4:d * 4 + 4], cs[:, d * 4:d * 4 + 4], -4.0, None, ALU.mult, None, [("cs", d)], [("hcs", d)])
        ts("dve", hb, vecs[:, 24:48], 0.5, None, ALU.mult, None, ["vecs"], ["hb"])
        for i in range(20):
            ts("dve", ldiag[:, i, :], ident_b, vcol(V_W5 + i), None, ALU.mult, None, ["ident_b", "vecs"], ["ldiag"])

        for kt in range(8):
            dma("pool", w_in_b[:, kt, :], w_in[kt * 128:(kt + 1) * 128, :], [], ["w_in_b"], "win", last=(kt == 7))
        mset("pool", zl_buf[:, :, 0:2], 0.0, ["zl_pad0"])
        mset("pool", zl_buf[:, :, 4098:4100], 0.0, ["zl_pad1"])
        mset("pool", zc_buf[:, :, 0:2], 0.0, ["zc_pad0"])
        mset("pool", zc_buf[:, :, 258:260], 0.0, ["zc_pad1"])
        mset("pool", upad.rearrange("p a b c -> p (a b c)"), 0.0, [("upad", c) for c in range(4)])
        for ct in range(16):
            for kt in range(8):
                mm(psM[:, 96 + ct * 2:98 + ct * 2], w_in_b[:, kt, ct * 128:(ct + 1) * 128], sh1_b[:, kt, :],
                   kt == 0, kt == 7, ["w_in_b", "sh1_b"], [PK(7)])
        cp("dve", zbias, psM[:, 96:128], [PK(7)], ["zbias"])

        def zb(ct, r):
            return zbias[:, ct * 2 + r:ct * 2 + r + 1]

        def norm_front(src, n0, N, par):
            dma("sp", xc[:, :, 0:N], src.rearrange("(t p) n -> p t n", p=128)[:, :, n0:n0 + N], [], ["xc"], "xc")
            act(sqb[:, :, 0:N], xc[:, :, 0:N], AF.Square, ["xc"], ["sqb"])
            for t in range(8):
                mm(PSB[6][:, 0:N], ones_b, sqb[:, t, 0:N], t == 0, t == 7, ["sqb", "ones_b"], [PK(6)])
            rt = rtb[par]
            act(rt[:, 0:N], PSB[6][:, 0:N], AF.Sqrt, [PK(6), "eps"], [("rtb", par)], bias=eps_t, scale=1.0 / 1024.0)
            recip(rt[:, 0:N], rt[:, 0:N], [("rtb", par)], [("rtb", par)])

        def norm_back(N, r, par):
            for t in range(8):
                stt("dve", sqhm[:, t, 0:N], xc[:, t, 0:N], gs1[:, t, r:r + 1], rtb[par][:, 0:N],
                    ALU.mult, ALU.mult, ["xc", "gs1", ("rtb", par)], [("sqhm", t)])

        def inproj(ct, N, bank):
            for kt in range(8):
                mm(PSB[bank][:, 0:N], w_in_b[:, kt, ct * 128:(ct + 1) * 128], sqhm[:, kt, 0:N], kt == 0, kt == 7,
                   [("sqhm", kt), "w_in_b"], [PK(bank)])
            return PSB[bank][:, 0:N], PK(bank)

        cx = {}
        for nm in ("ul", "rr", "ii", "aa"):
            cx[nm] = [tmp[i] for i in range(len(cx) * 2, len(cx) * 2 + 2)]

        def lru_steps(ulf, ulk, ulb, ubk, rr, ii, aa, key, d, c4s, N, inits, ikeys, reverse, b1, b2):
            for c4 in c4s:
                mm(PSB[b1][:, 0:N], lbd[:, (d * 2 + 0) * 4 + c4, :], ulb[c4][:, 0:N], True, True, [ubk(c4), "lbd"], [PK(b1)])
                mm(PSB[b2][:, 0:N], lbd[:, (d * 2 + 1) * 4 + c4, :], ulb[c4][:, 0:N], True, True, [ubk(c4), "lbd"], [PK(b2)])
                act(rr[c4][:, 0:N], PSB[b1][:, 0:N], AF.Tanh, [PK(b1), "hb"], [key("rr", c4)],
                    bias=hb[:, 12 * d + c4:12 * d + c4 + 1], scale=0.5)
                act(ii[c4][:, 0:N], PSB[b2][:, 0:N], AF.Tanh, [PK(b2), "hb"], [key("ii", c4)],
                    bias=hb[:, 12 * d + 4 + c4:12 * d + 5 + c4], scale=0.5)
            for c4 in c4s:
                hc = hcs[:, d * 4 + c4:d * 4 + c4 + 1]
                act(aa[c4][:, 0:N], rr[c4][:, 0:N], AF.Exp, [key("rr", c4), ("hcs", d)], [key("aa", c4)], bias=hc, scale=hc)
            for c4 in c4s:
                stt("dve", rr[c4][:, 0:N], aa[c4][:, 0:N], -1.0, aa[c4][:, 0:N], ALU.mult, ALU.mult, [key("aa", c4)], [key("rr", c4)])
                ts("dve", rr[c4][:, 0:N], rr[c4][:, 0:N], 1.0, 1e-12, ALU.add, ALU.max, [key("rr", c4)], [key("rr", c4)])
            for c4 in c4s:
                act(rr[c4][:, 0:N], rr[c4][:, 0:N], AF.Sqrt, [key("rr", c4)], [key("rr", c4)], scale=0.25)

        def lru_back(ulf, ulk, rr, ii, aa, key, c4s, N, inits, ikeys, reverse):
            for c4 in c4s:
                tt("pool", rr[c4][:, 0:N], rr[c4][:, 0:N], ulf[c4][:, 0:N], ALU.mult, [key("rr", c4), ulk(c4)], [key("rr", c4)])
            for c4 in c4s:
                stt("dve", ii[c4][:, 0:N], ii[c4][:, 0:N], 1.0, rr[c4][:, 0:N], ALU.add, ALU.mult,
                    [key("ii", c4), key("rr", c4)], [key("ii", c4)])
            for c4 in c4s:
                o_, a_, b_ = ii[c4][:, 0:N], aa[c4][:, 0:N], ii[c4][:, 0:N]
                if reverse:
                    o_, a_, b_ = o_[:, ::-1], a_[:, ::-1], b_[:, ::-1]
                S.op("dve", (lambda o__, a__, b__, i__: (lambda e: e.tensor_tensor_scan(
                    out=o__, data0=a__, data1=b__, initial=i__, op0=ALU.mult, op1=ALU.add)))(o_, a_, b_, inits[c4]),
                    [key("aa", c4), key("ii", c4)] + ikeys(c4), [key("ii", c4)])

        def lconv_all(buf, bkeys, c4s, m0, N, ulf, ulk, ulb, ubk, bank):
            for c4 in c4s:
                for tap in range(5):
                    mm(PSB[bank][:, 0:N], ldiag[:, c4 * 5 + tap, :], buf[:, c4, m0 + tap:m0 + tap + N], tap == 0, tap == 4,
                       bkeys(c4) + ["ldiag"], [PK(bank)])
                act(ulf[c4][:, 0:N], PSB[bank][:, 0:N], AF.Identity, [PK(bank), "vecs"], [ulk(c4)], bias=vcol(V_LCB + c4))
                cp("dve", ulb[c4][:, 0:N], ulf[c4][:, 0:N], [ulk(c4)], [ubk(c4)])

        norm_front(ctxT, 0, 256, 0)
        norm_back(256, 1, 0)
        for c4 in range(4):
            ps, pk = inproj(8 + c4, 256, c4 % 4)
            act(zc_buf[:, c4, 2:258], ps, AF.Identity, [pk, "zbias"], [("zc", c4)], bias=zb(8 + c4, 1))
        norm_front(xT, 4 * 512, 512, 1)
        c_ul = {0: tmp[0], 1: tmp[1]}
        c_ub = {0: tmp[2].bitcast(BF16), 1: tmp[2].bitcast(BF16)[:, 512:1024]}
        c_rr = {0: tmp[3], 1: tmp[4]}
        c_ii = {0: tmp[5], 1: tmp[6]}
        c_aa = {0: tmp[7], 1: tmp[8]}
        for pair in range(2):
            for d in range(2):
                loc = {0: 0, 1: 1}
                c4s = [pair * 2, pair * 2 + 1]
                ulf = {c4: c_ul[c4 % 2] for c4 in c4s}
                ulb = {c4: c_ub[c4 % 2] for c4 in c4s}
                rr = {c4: c_rr[c4 % 2] for c4 in c4s}
                ii = {c4: c_ii[c4 % 2] for c4 in c4s}
                aa = {c4: c_aa[c4 % 2] for c4 in c4s}
                key = lambda n, c4: ("tmp", {"rr": 3, "ii": 5, "aa": 7}[n] + c4 % 2)
                ulk = lambda c4: ("tmp", c4 % 2)
                ubk = lambda c4: ("tmp", 2)
                S.reg
                lconv_all(zc_buf, lambda c4: [("zc", c4), "zc_pad0", "zc_pad1"], c4s, 0, 256, ulf, ulk, ulb, ubk, 4)
                lru_steps(ulf, ulk, ulb, ubk, rr, ii, aa, key, d, c4s, 256, None, None, d == 1, 0, 1)
                lru_back(ulf, ulk, rr, ii, aa, key, c4s, 256, {c4: 0.0 for c4 in c4s}, lambda c4: [], d == 1)
                for c4 in c4s:
                    src = ii[c4][:, 255:256] if d == 0 else ii[c4][:, 0:1]
                    cp("pool", carry[:, d * 4 + c4:d * 4 + c4 + 1], src, [key("ii", c4)], [("carry", d, c4)])
        tctr[0] = 0

        norm_back(512, 0, 1)
        for k in range(4, 8):
            nxt = k + 1 if k < 7 else 0
            for c4 in range(4):
                ps, pk = inproj(8 + c4, 512, c4 % 4)
                act(zl_buf[:, c4, 2 + k * 512:2 + (k + 1) * 512], ps, AF.Identity, [pk, "zbias"], [("zl", c4, k)],
                    bias=zb(8 + c4, 0))
                if c4 == 1:
                    norm_front(xT, nxt * 512, 512, k % 2)
            norm_back(512, 0, k % 2)
            cv_next(1)

        for k in range(4):
            sl = slice(k * 512, (k + 1) * 512)
            for c4 in range(4):
                ps, pk = inproj(8 + c4, 512, c4 % 4)
                act(zl_buf[:, c4, 2 + k * 512:2 + (k + 1) * 512], ps, AF.Identity, [pk, "zbias"], [("zl", c4, k)],
                    bias=zb(8 + c4, 0))
            gz = []
            for c4 in range(4):
                ps, pk = inproj(12 + c4, 512, c4 % 4)
                zg, zgk = T()
                t1, t1k = T()
                act(zg, ps, AF.Identity, [pk, "zbias"], [zgk], bias=zb(12 + c4, 0))
                gz.append((zg, zgk, t1, t1k))
            for zg, zgk, t1, t1k in gz:
                tt("pool", t1, zg, zg, ALU.mult, [zgk], [t1k])
            for zg, zgk, t1, t1k in gz:
                ts("dve", t1, t1, 0.044715, 1.0, ALU.mult, ALU.add, [t1k], [t1k])
            for zg, zgk, t1, t1k in gz:
                tt("pool", t1, t1, zg, ALU.mult, [t1k, zgk], [t1k])
            for zg, zgk, t1, t1k in gz:
                act(t1, t1, AF.Sigmoid, [t1k], [t1k], scale=1.5957691216057308)
            for c4, (zg, zgk, t1, t1k) in enumerate(gz):
                tt("pool", gel[:, c4, sl], t1, zg, ALU.mult, [t1k, zgk], [("gel", c4, k)])
            if k < 3:
                norm_front(xT, (k + 1) * 512, 512, k % 2)

            def stageA(c4):
                bv, bg = 2 * (c4 % 2), 2 * (c4 % 2) + 1
                psv, pkv = inproj(c4, 512, bv)
                psg, pkg = inproj(4 + c4, 512, bg)
                val, valk = T()
                sg, sgk = T()
                act(val, psv, AF.Identity, [pkv, "zbias"], [valk], bias=zb(c4, 0))
                act(sg, psg, AF.Sigmoid, [pkg, "zbias"], [sgk], bias=zb(4 + c4, 0))
                tt("dve", upad[:, c4, :, 15:79], val.rearrange("p (a b) -> p a b", a=8),
                   sg.rearrange("p (a b) -> p a b", a=8), ALU.mult, [valk, sgk], [("upad", c4)])
                cb = c4 % 2
                for tap in range(31):
                    wcol = cdw[:, c4 * 31 + tap:c4 * 31 + tap + 1]
                    if tap % 3 == 2:
                        act(cdg[cb][:, tap, :], ident_b, AF.Copy, ["ident_b", "cdw"], [("cdg", cb, tap)], scale=wcol)
                    else:
                        ts("dve", cdg[cb][:, tap, :], ident_b, wcol, None, ALU.mult, None,
                           ["ident_b", "cdw"], [("cdg", cb, tap)])

            def stageB(c4):
                cb = c4 % 2
                bank = 4 + (c4 % 2)
                pcv = PSB[bank].rearrange("p (a b) -> p a b", a=8)
                for tap in range(31):
                    mm(pcv, cdg[cb][:, tap, :], upad[:, c4, :, tap:tap + 64], tap == 0, tap == 30,
                       [("cdg", cb, tap), ("upad", c4)], [PK(bank)])
                act(cc[:, c4, :], PSB[bank], AF.Identity, [PK(bank), "vecs"], [("cc", c4)], bias=vcol(V_CB + c4))
                act(csq[:, c4, :], cc[:, c4, :], AF.Square, [("cc", c4)], [("csq", c4)])
                cp("dve", cbf[:, c4, :], cc[:, c4, :], [("cc", c4)], [("cbf", c4)])

            cv_next(1)
            stageA(0)
            stageA(1)
            cv_next(1)
            stageB(0)
            stageA(2)
            cv_next(1)
            stageB(1)
            stageA(3)
            cv_next(2)
            stageB(2)
            stageB(3)
            for c4 in range(4):
                mm(PSB[6], ones512_b, cbf[:, c4, :], c4 == 0, c4 == 3, [("cbf", c4), "ones512_b"], [PK(6)])
            for c4 in range(4):
                mm(PSB[7], ones512_b, csq[:, c4, :], c4 == 0, c4 == 3, [("csq", c4), "ones512_b"], [PK(7)])
            m2, m2k = T()
            act(m2, PSB[6], AF.Square, [PK(6)], [m2k])
            tt("dve", m2, PSB[7], m2, ALU.subtract, [PK(7), m2k], [m2k])
            ts("dve", m2, m2, 0.0, None, ALU.max, None, [m2k], [m2k])
            act(m2, m2, AF.Sqrt, [m2k, "eps"], [m2k], bias=eps_t)
            recip(m2, m2, [m2k], [m2k])
            for c4 in range(4):
                dd, ddk = T()
                tt("dve", dd, cc[:, c4, :], PSB[6], ALU.subtract, [("cc", c4), PK(6)], [ddk])
                tt("pool", dd, dd, m2, ALU.mult, [ddk, m2k], [ddk])
                act(convT[:, c4, sl], dd, AF.Silu, [ddk, "vecs"], [("convT", c4, k)],
                    bias=vcol(V_LNB + c4), scale=vcol(V_LNG + c4))
            if k < 3:
                norm_back(512, 0, k % 2)

        S.barrier()
        AR.off = markM

        g1_bc = AR.alloc([1024], F32)
        gs2_bc = AR.alloc([1024], F32)
        sh2_bc = AR.alloc([1024], F32)
        wr_sb = AR.alloc([8, 36], F32)
        br_sb = AR.alloc([36], F32)
        dgt = [AR.alloc([128], F32) for _ in range(2)]
        hB_buf = AR.alloc([4, 2048], BF16)
        w_out_b = AR.alloc([8, 1024], BF16)
        mixl = AR.alloc([4, 512], BF16)
        xx_all = AR.alloc([4, 1024], F32)
        xt_t = [xx_all[:, 0, :], xx_all[:, 1, :]]
        x1_t = [xx_all[:, 2, :], xx_all[:, 3, :]]
        ztile2 = AR.alloc([512], BF16)
        h2_t = [AR.alloc([1024], F32) for _ in range(2)]
        h2T_tt = [AR.alloc([1024], F32) for _ in range(2)]
        h2b_t = [AR.alloc([1024], BF16) for _ in range(2)]
        ssq = AR.alloc([16], F32)
        m_ul = [AR.alloc([512], F32) for _ in range(4)]
        m_ub = [AR.alloc([512], BF16) for _ in range(4)]
        m_rr = [AR.alloc([512], F32) for _ in range(4)]
        m_ii = [AR.alloc([512], F32) for _ in range(4)]
        m_aa = [AR.alloc([512], F32) for _ in range(4)]
        wabf = xx_all.rearrange("p a b -> p (a b)").rearrange("p (t n) -> p t n", t=8)
        wabfk = [("xt", 0), ("xt", 1), ("x1", 0, 0), ("x1", 0, 1), ("x1", 1, 0), ("x1", 1, 1)]
        print("M2 arena bytes", AR.off)

        for kt in range(8):
            dma("pool", w_out_b[:, kt, :], w_out[kt * 128:(kt + 1) * 128, :], [], ["w_out_b"], "wout", last=(kt == 7))
        dma("sp", br_sb, br_d, [], ["br"], "c2")
        dma("sp", wr_sb, wr_d.rearrange("(t p) n -> p t n", p=128), [], ["wr"], "c3")
        dma("sp", gs2_bc, n2g_d, [], ["gs2_bc"], "c4")
        mset("pool", ssq, 0.0, [("ssq", j) for j in range(16)])
        def ada_dma(c):
            dma("sp", wabf, w_ada.rearrange("(t p) n -> p t n", p=128)[:, :, c * 512:(c + 1) * 512],
                [], wabfk, "wabf")

        def ada_mm(c, col0):
            for ctl in range(4):
                ct = c * 4 + ctl
                for kt in range(8):
                    mm(psM[:, (ct - col0) * 2:(ct - col0) * 2 + 2], wabf[:, kt, ctl * 128:(ctl + 1) * 128],
                       scTf[:, kt, :], kt == 0, kt == 7, wabfk + ["scTf"], [PK(7)])

        mset("dve", ztile2, 0.0, ["ztile2"])
        xs_flat = xs_d.rearrange("(p a) d -> p (a d)", p=128)
        NZ = NR * 1024 // 128 // 512
        zf_pos = [0]

        def zf_next(n):
            for _ in range(n):
                i = zf_pos[0]
                if i >= NZ:
                    return
                zf_pos[0] += 1
                dma("sp", xs_flat[:, i * 512:(i + 1) * 512], ztile2, ["ztile2"], ["xs_d"], "zf", last=(i == NZ - 1))

        def bcast_row(v, dst, key, post):
            for t in range(8):
                dg = dgt[t % 2]
                ts("dve", dg, ident_f, modv(v, t, 0), None, ALU.mult, None, ["ident_f", "mod_b"], [("dgt", t % 2)])
                bank = 5 + t // 4
                mm(PSB[bank][:, (t % 4) * 128:(t % 4 + 1) * 128], ones_f, dg, True, True,
                   [("dgt", t % 2), "ones_f"], [PK(bank)])
            for h in range(2):
                post(dst[:, h * 512:(h + 1) * 512], PSB[5 + h], [PK(5 + h)], [key])

        mkey = lambda n, c4: ("m" + n, c4)
        ulk = lambda c4: ("mul", c4)
        ubk = lambda c4: ("mub", c4)
        D4 = {c4: c4 for c4 in range(4)}
        ulf = {c4: m_ul[c4] for c4 in range(4)}
        ulb = {c4: m_ub[c4] for c4 in range(4)}
        rr = {c4: m_rr[c4] for c4 in range(4)}
        ii = {c4: m_ii[c4] for c4 in range(4)}
        aa = {c4: m_aa[c4] for c4 in range(4)}
        C4 = [0, 1, 2, 3]

        def zlkeys(k):
            return lambda c4: [("zl", c4, kk) for kk in range(max(0, k - 1), min(8, k + 2))] + ["zl_pad0", "zl_pad1"]

        for k in range(7, -1, -1):
            lconv_all(zl_buf, zlkeys(k), C4, k * 512, 512, ulf, ulk, ulb, ubk, 0)
            lru_steps(ulf, ulk, ulb, ubk, rr, ii, aa, mkey, 1, C4, 512, None, None, True, 1, 2)
            lru_back(ulf, ulk, rr, ii, aa, mkey, C4, 512, {c4: carry[:, 4 + c4:5 + c4] for c4 in C4},
                     lambda c4: [("carry", 1, c4)], True)
            for c4 in C4:
                cp("pool", carry[:, 4 + c4:5 + c4], ii[c4][:, 0:1], [mkey("ii", c4)], [("carry", 1, c4)])
                if k < 4:
                    cp("pool", hB_buf[:, c4, k * 512:(k + 1) * 512], ii[c4], [mkey("ii", c4)], [("hB", c4, k)])
            step = 7 - k
            if step >= 1:
                ada_mm(4 + step - 1, 16)
            ada_dma(4 + step)
            zf_next(24)
            cv_next(2)
        ada_mm(11, 16)
        tt("dve", mod_fm[:, 32:96], psM[:, 0:64], bada[:, 32:96], ALU.add, [PK(7), "bada"], ["mod_b"])
        bcast_row(2, g1_bc, "g1_bc", lambda o, p, r, w: cp("dve", o, p, r, w))
        bcast_row(3, sh2_bc, "sh2_bc", lambda o, p, r, w: cp("dve", o, p, r, w))
        bcast_row(4, gs2_bc, "gs2_bc",
                  lambda o, p, r, w: stt("dve", o, p, 1.0, o, ALU.add, ALU.mult, r + ["gs2_bc"], w))
        for kt in range(8):
            tt("pool", w_out_b[:, kt, :], w_out_b[:, kt, :], g1_bc, ALU.mult, ["w_out_b", "g1_bc"], ["w_out_b"])


        def tokX(k, tl):
            j = k * 4 + tl
            b = j % 2
            xt, x1, h2, h2b = xt_t[b], x1_t[b], h2_t[b], h2b_t[b]
            dma("sp", xt, xtok[j * 128:(j + 1) * 128, :], [], [("xt", b)], "xt%d" % b)
            for h in range(2):
                bank = 3 + h
                for kt in range(8):
                    if kt < 4:
                        lhs = convT[:, kt, j * 128:(j + 1) * 128]
                        rk_ = [("convT", kt, k)]
                    else:
                        lhs = mixl[:, kt - 4, tl * 128:(tl + 1) * 128]
                        rk_ = [("mixl", kt - 4)]
                    mm(PSB[bank], lhs, w_out_b[:, kt, h * 512:(h + 1) * 512], kt == 0, kt == 7,
                       rk_ + ["w_out_b"], [PK(bank)])
                hs = slice(h * 512, (h + 1) * 512)
                tt("dve", x1[:, hs], PSB[bank], xt[:, hs], ALU.add, [PK(bank), ("xt", b)], [("x1", b, h)])
            x1k = [("x1", b, 0), ("x1", b, 1)]
            dma("sp", x1_d[j * 128:(j + 1) * 128, :], x1, x1k, [("x1_d", j)], "x1s%d" % b)
            act(h2, x1, AF.Square, x1k, [("h2", b), ("ssq", j)], accum=ssq[:, j:j + 1])
            act(ssq[:, j:j + 1], ssq[:, j:j + 1], AF.Sqrt, [("ssq", j), "eps"], [("ssq", j)], bias=eps_t, scale=1.0 / 1024.0)
            recip(ssq[:, j:j + 1], ssq[:, j:j + 1], [("ssq", j)], [("ssq", j)])
            stt("dve", h2, x1, ssq[:, j:j + 1], gs2_bc, ALU.mult, ALU.mult, x1k + [("ssq", j), "gs2_bc"], [("h2", b)])
            tt("dve", h2, h2, sh2_bc, ALU.add, [("h2", b), "sh2_bc"], [("h2", b)])
            cp("act", h2b, h2, [("h2", b)], [("h2b", b)])
            dma("sp", h2_d[j * 128:(j + 1) * 128, :], h2b, [("h2b", b)], [("h2_d", j)], "h2s%d" % b)

        def tokYt(k, tl):
            j = k * 4 + tl
            b = j % 2
            h2 = h2_t[b]
            h2T_t = h2T_tt[b]
            for kt in range(8):
                bank = 5 + kt // 4
                S.op("pe", (lambda o_, i_: (lambda e: e.transpose(out=o_, in_=i_, identity=ident_f)))(
                    PSB[bank][:, (kt % 4) * 128:(kt % 4 + 1) * 128], h2[:, kt * 128:(kt + 1) * 128]),
                    [("h2", b), "ident_f"], [PK(bank)])
            cp("act", h2T_t[:, 0:512], PSB[5], [PK(5)], [("h2T", b, 0)])
            cp("dve", h2T_t[:, 512:1024], PSB[6], [PK(6)], [("h2T", b, 1)])

        def tokYl(k, tl):
            j = k * 4 + tl
            b = j % 2
            h2T_t = h2T_tt[b]
            for kt in range(8):
                mm(PSB[7][:, 0:36], h2T_t[:, kt * 128:(kt + 1) * 128], wr_sb[:, kt, :], kt == 0, kt == 7,
                   [("h2T", b, kt // 4), "wr"], [PK(7)])
            tt("dve", lg_all[:, j, :], PSB[7][:, 0:36], br_sb, ALU.add, [PK(7), "br"], [("lg", j)])

        def token_stage(k):
            tokX(k, 0)
            tokX(k, 1)
            tokYt(k, 0)
            tokX(k, 2)
            tokYt(k, 1)
            tokYl(k, 0)
            tokX(k, 3)
            tokYt(k, 2)
            tokYl(k, 1)
            tokYt(k, 3)
            tokYl(k, 2)
            tokYl(k, 3)

        for k in range(4):
            lconv_all(zl_buf, zlkeys(k), C4, k * 512, 512, ulf, ulk, ulb, ubk, 0)
            lru_steps(ulf, ulk, ulb, ubk, rr, ii, aa, mkey, 0, C4, 512, None, None, False, 1, 2)
            cv_next(3)
            if k > 0:
                token_stage(k - 1)
            lru_back(ulf, ulk, rr, ii, aa, mkey, C4, 512, {c4: carry[:, c4:c4 + 1] for c4 in C4},
                     lambda c4: [("carry", 0, c4)], False)
            for c4 in C4:
                cp("pool", carry[:, c4:c4 + 1], ii[c4][:, 511:512], [mkey("ii", c4)], [("carry", 0, c4)])
                tt("pool", ii[c4], ii[c4], hB_buf[:, c4, k * 512:(k + 1) * 512], ALU.add, [mkey("ii", c4), ("hB", c4, k)],
                   [mkey("ii", c4)])
                tt("pool", mixl[:, c4, :], ii[c4], gel[:, c4, k * 512:(k + 1) * 512], ALU.mult,
                   [mkey("ii", c4), ("gel", c4, k)], [("mixl", c4)])
        token_stage(3)
        cv_next(len(cv_list))

        S.barrier()
        AR.off = MOEBASE
        if stage == 1:
            for j in range(16):
                dma("sp", out[j * 128:(j + 1) * 128, :], x1_d[j * 128:(j + 1) * 128, :], [("x1_d", j)], [("out", j)], "os0")
            S.final_wait("sp", [("out", j) for j in range(16)])
        else:
            build_moe(nc, S, AR, PSB, locals())
        S.emit()
    return nc


def build_moe(nc, S, AR, PSB, L):
    (mm, act, tt, ts, stt, cp, recip, mset, dma, red) = (L[k] for k in
                                                         ("mm", "act", "tt", "ts", "stt", "cp", "recip", "mset", "dma", "red"))
    lg_all, ones_b, U_b, ident_b, ident_f, ones_f, eps_t, mod_fm = (L[k] for k in (
        "lg_all", "ones_b", "U_b", "ident_b", "ident_f", "ones_f", "eps_t", "mod_fm"))
    x1_d, h2_d, xs_d, y_d, out, fg_d = (L[k] for k in ("x1_d", "h2_d", "xs_d", "y_d", "out", "fg_d"))
    wbf = L["wbf"]
    w1, w3, w2 = L["w1"], L["w3"], L["w2"]
    modv = L["modv"]

    def PK(i):
        return ("ps", i)

    A = AR.alloc
    lgk = [("lg", j) for j in range(16)]
    gate = A([16, 2], F32)
    dest_i = A([16, 2], I32)
    idx_i = A([NSLOT], I32)
    idxB_i = A([NSLOT], I32)
    g2_bc = A([1024], F32)
    fg_bc = A([1024], F32)
    mark_r = AR.off
    gmax = A([16], F32)
    oh = A([16, 4], F32)
    d4 = A([16, 4], F32)
    pg = A([16], F32)
    sel = A([16, 32], F32)
    sel2 = A([16, 32], F32)
    mask1 = A([16, 32], F32)
    mask2 = A([16, 32], F32)
    m1 = A([16], F32)
    m2 = A([16], F32)
    Mb = A([512], BF16)
    tcnt = A([16, 32], F32)
    rank = A([16, 32], F32)
    off = A([16, 32], F32)
    cnt = A([32], F32)
    cnt_i = A([32], I32)
    padded = A([32], F32)
    pend = A([32], F32)
    pstart = A([32], F32)
    ones32 = A([32], F32)
    pos = A([16, 32], F32)
    pm = A([16, 32], F32)
    dest_f = A([16, 2], F32)
    sbs = A([NSLOT], F32)
    cmp = A([NSLOT, 32], F32)
    blk = A([NSLOT], F32)
    base8 = A([1], F32)
    idx_f = A([NSLOT], F32)
    idxA_f = A([NSLOT], F32)
    same = A([NSLOT], F32)
    pmask = A([1], F32)
    dgt = [A([128], F32) for _ in range(2)]

    R = "rt"
    lgv = lg_all[:, :, 0:4]
    lev = lg_all[:, :, 4:36]
    red("dve", gmax, lgv, ALU.max, lgk, [R])
    tt("dve", oh, lgv, gmax.unsqueeze(2).to_broadcast([128, 16, 4]), ALU.is_equal, lgk + [R], [R])
    tt("dve", d4, lgv, gmax.unsqueeze(2).to_broadcast([128, 16, 4]), ALU.subtract, lgk + [R], [R])
    act(d4, d4, AF.Exp, [R], [R])
    red("dve", pg, d4, ALU.add, [R], [R])
    recip(pg, pg, [R], [R])
    ts("dve", oh, oh, 1e30, -1e30, ALU.mult, ALU.add, [R], [R])
    tt("dve", sel.rearrange("p j (g e) -> p j g e", g=4), lev.rearrange("p j (g e) -> p j g e", g=4),
       oh.unsqueeze(3).to_broadcast([128, 16, 4, 8]), ALU.add, lgk + [R], [R])
    red("dve", m1, sel, ALU.max, [R], [R])
    tt("dve", mask1, sel, m1.unsqueeze(2).to_broadcast([128, 16, 32]), ALU.is_equal, [R], [R])
    stt("dve", sel2, mask1, -1e30, sel, ALU.mult, ALU.add, [R], [R])
    red("dve", m2, sel2, ALU.max, [R], [R])
    tt("dve", mask2, sel2, m2.unsqueeze(2).to_broadcast([128, 16, 32]), ALU.is_equal, [R], [R])
    tt("dve", m2, m2, m1, ALU.subtract, [R], [R])
    act(m2, m2, AF.Exp, [R], [R])
    ts("dve", m2, m2, 1.0, None, ALU.add, None, [R], [R])
    recip(m2, m2, [R], [R])
    tt("dve", gate[:, :, 0], pg, m2, ALU.mult, [R], [R])
    tt("dve", gate[:, :, 1], pg, gate[:, :, 0], ALU.subtract, [R], [R])
    tt("dve", Mb.rearrange("p (j e) -> p j e", j=16), mask1, mask2, ALU.add, [R], [R])
    mm(PSB[0], ones_b, Mb, True, True, [R, "ones_b"], [PK(0)])
    mm(PSB[1], U_b, Mb, True, True, [R, "U_b"], [PK(1)])
    cp("dve", tcnt.rearrange("p j e -> p (j e)"), PSB[0], [PK(0)], [R])
    cp("dve", rank.rearrange("p j e -> p (j e)"), PSB[1], [PK(1)], [R])
    mset("dve", off[:, 0, :], 0.0, [R])
    for j in range(1, 16):
        tt("dve", off[:, j, :], off[:, j - 1, :], tcnt[:, j - 1, :], ALU.add, [R], [R])
    tt("dve", cnt, off[:, 15, :], tcnt[:, 15, :], ALU.add, [R], [R])
    cp("dve", cnt_i, cnt, [R], [R])
    ts("dve", cnt_i, cnt_i, BS - 1, None, ALU.add, None, [R], [R])
    ts("dve", cnt_i, cnt_i, 8, None, ALU.arith_shift_right, None, [R], [R])
    ts("dve", cnt_i, cnt_i, 8, None, ALU.logical_shift_left, None, [R], [R])
    cp("dve", padded, cnt_i, [R], [R])
    mset("dve", ones32, 1.0, [R])
    S.op("dve", lambda e: e.tensor_tensor_scan(out=pend, data0=ones32, data1=padded, initial=0.0,
                                               op0=ALU.mult, op1=ALU.add), [R], [R])
    tt("dve", pstart, pend, padded, ALU.subtract, [R], [R])
    tt("dve", pos, rank, off, ALU.add, [R], [R])
    tt("dve", pos, pos, pstart.unsqueeze(1).to_broadcast([128, 16, 32]), ALU.add, [R], [R])
    tt("dve", pm, pos, mask1, ALU.mult, [R], [R])
    red("dve", dest_f[:, :, 0], pm, ALU.add, [R], [R])
    tt("dve", pm, pos, mask2, ALU.mult, [R], [R])
    red("dve", dest_f[:, :, 1], pm, ALU.add, [R], [R])
    cp("dve", dest_i, dest_f, [R], [R])
    S.op("pool", lambda e: e.iota(sbs, pattern=[[BS, NSLOT]], base=0, channel_multiplier=0,
                                  allow_small_or_imprecise_dtypes=True), [], ["sbs"])
    S.op("pool", lambda e: e.iota(base8, pattern=[[0, 1]], base=0, channel_multiplier=1,
                                  allow_small_or_imprecise_dtypes=True), [], ["base8"])
    tt("dve", cmp, pend.unsqueeze(1).to_broadcast([128, NSLOT, 32]), sbs.unsqueeze(2).to_broadcast([128, NSLOT, 32]),
       ALU.is_le, [R, "sbs"], [R])
    red("dve", blk, cmp, ALU.add, [R], [R])
    ts("dve", blk, blk, 31.0, 128.0, ALU.min, ALU.mult, [R], [R])
    ts("dve", idx_f, blk, base8[:, 0:1], None, ALU.add, None, [R, "base8"], [R])
    S.op("pool", lambda e: e.iota(pmask, pattern=[[0, 1]], base=0, channel_multiplier=1,
                                  allow_small_or_imprecise_dtypes=True), [], ["pmask"])
    ts("dve", pmask, pmask, 0.5, None, ALU.is_gt, None, ["pmask"], ["pmask"])
    mset("dve", same, 0.0, [R])
    H = NSLOT // 2
    tt("dve", same[:, 2:H], blk[:, 2:H], blk[:, 0:H - 2], ALU.is_equal, [R], [R])
    tt("dve", same[:, H:NSLOT - 1], blk[:, H:NSLOT - 1], blk[:, H + 1:NSLOT], ALU.is_equal, [R], [R])
    ts("dve", same, same, pmask[:, 0:1], 4.0e6, ALU.mult, ALU.mult, [R, "pmask"], [R])
    tt("dve", idx_f, idx_f, same, ALU.add, [R], [R])
    cp("dve", idx_i, idx_f, [R], [R])
    OFF = float((32 - NCV) * 128)
    ts("dve", same, blk, OFF - 0.5, 4.0e6, ALU.is_lt, ALU.mult, [R], [R])
    stt("dve", idxA_f, idx_f, -OFF, same, ALU.add, ALU.add, [R], [R])
    cp("dve", idx_i, idxA_f, [R], [R])
    ts("dve", same, same, -1.0, 4.0e6, ALU.mult, ALU.add, [R], [R])
    tt("dve", idx_f, idx_f, same, ALU.add, [R], [R])
    cp("dve", idxB_i, idx_f, [R], [R])

    dma("sp", fg_bc, fg_d, [], ["fg_bc"], "c0")
    for t in range(8):
        dg = dgt[t % 2]
        ts("dve", dg, ident_f, modv(5, t, 0), None, ALU.mult, None, ["ident_f", "mod_fm"], [("dgt", t % 2)])
        bank = 5 + t // 4
        mm(PSB[bank][:, (t % 4) * 128:(t % 4 + 1) * 128], ones_f, dg, True, True, [("dgt", t % 2), "ones_f"], [PK(bank)])
    for h in range(2):
        cp("dve", g2_bc[:, h * 512:(h + 1) * 512], PSB[5 + h], [PK(5 + h)], ["g2_bc"])

    S.barrier()
    AR.off = mark_r
    hall = A([16, 1024], BF16)
    dma("sp", hall, h2_d.rearrange("(j p) d -> p j d", p=128), [("h2_d", j) for j in range(16)], ["hall"], "hall")
    for j in range(16):
        for k in range(2):
            S.dma("pool", (lambda j_, k_: (lambda e: e.indirect_dma_start(
                out=xs_d[:, :], out_offset=bass.IndirectOffsetOnAxis(ap=dest_i[:, j_, k_:k_ + 1], axis=0),
                in_=hall[:, j_, :], in_offset=None)))(j, k),
                ["hall", R], ["xs_d"], sem="scat", last=(j == 15 and k == 1))

    S.barrier()
    AR.off = mark_r
    Wb = [[A([8, 1024], BF16) for _ in range(3)] for _ in range(3)]
    xtm = [A([2, 1024], BF16) for _ in range(2)]
    XeT = [A([8, 256], BF16) for _ in range(2)]
    aT = [A([8, 256], BF16) for _ in range(1)]
    ysb = [A([2, 1024], F32) for _ in range(1)]
    print("MoE arena bytes", AR.off)
    slt = [A([256], F32) for _ in range(2)]
    wd = [w1, w3, w2]
    bc_cache = {}

    def bc_reg(e, val):
        if val not in bc_cache:
            bc_cache[val] = e.to_reg(val)
        return bc_cache[val]

    order = []
    for i in range(NSLOT // 2):
        order.append((i, i % 2))
        order.append((NSLOT - 1 - i, 2))
    for oi, (s, wbuf) in enumerate(order):
        wb = oi % 2
        for m in range(3):
            S.dma("pool", (lambda m_, s_, wb_: (lambda e: e.indirect_dma_start(
                out=Wb[wb_][m_].rearrange("p a b -> p (a b)"), out_offset=None,
                in_=wbf[m_].rearrange("(r k) n -> r (k n)", k=8),
                in_offset=bass.IndirectOffsetOnAxis(ap=idx_i[:, s_:s_ + 1], axis=0),
                bounds_check=bc_reg(e, NCV * 128 - 1), oob_is_err=False)))(m, s, wbuf),
                [R] + [("wbf", m, e_) for e_ in range(32 - NCV, 32)], [("W", wbuf, m)], sem="w%d%d" % (wbuf, m), last=False)
            S.dma("pool", (lambda m_, s_, wb_: (lambda e: e.indirect_dma_start(
                out=Wb[wb_][m_].rearrange("p a b -> p (a b)"), out_offset=None,
                in_=wd[m_].rearrange("(r k) n -> r (k n)", k=8),
                in_offset=bass.IndirectOffsetOnAxis(ap=idxB_i[:, s_:s_ + 1], axis=0),
                bounds_check=bc_reg(e, 4095), oob_is_err=False)))(m, s, wbuf),
                [R], [("W", wbuf, m)], sem="w%d%d" % (wbuf, m))
        if oi == 0:
            dma("sp", xtm[0], xs_d[s * BS:(s + 1) * BS, :].rearrange("(b p) d -> p b d", p=128), ["xs_d"], [("xtm", 0)], "xtm0")
        if oi + 1 < NSLOT:
            nb = (oi + 1) % 2
            s2 = order[oi + 1][0]
            dma("sp", xtm[nb], xs_d[s2 * BS:(s2 + 1) * BS, :].rearrange("(b p) d -> p b d", p=128), ["xs_d"],
                [("xtm", nb)], "xtm%d" % nb)
        for b in range(2):
            bank = b
            pst = PSB[bank].bitcast(BF16)
            for kt in range(8):
                S.op("pe", (lambda o_, i_: (lambda e: e.transpose(out=o_, in_=i_, identity=ident_b)))(
                    pst[:, kt * 128:(kt + 1) * 128], xtm[wb][:, b, kt:1024:8]),
                    [("xtm", wb), "ident_b"], [PK(bank)])
            cp("act" if b == 0 else "dve", XeT[wb][:, :, b * 128:(b + 1) * 128],
               pst.rearrange("p (k r) -> p k r", k=8), [PK(bank)], [("XeT", wb, b)])
        xk = [("XeT", wb, 0), ("XeT", wb, 1)]
        for ft in range(8):
            b1 = 2 + (ft % 2)
            b3 = 4 + (ft % 2)
            for kt in range(8):
                mm(PSB[b1][:, 0:BS], Wb[wbuf][0][:, kt, ft:1024:8], XeT[wb][:, kt, :], kt == 0, kt == 7,
                   xk + [("W", wbuf, 0)], [PK(b1)])
            for kt in range(8):
                mm(PSB[b3][:, 0:BS], Wb[wbuf][1][:, kt, ft:1024:8], XeT[wb][:, kt, :], kt == 0, kt == 7,
                   xk + [("W", wbuf, 1)], [PK(b3)])
            sl = slt[ft % 2]
            act(sl, PSB[b1][:, 0:BS], AF.Silu, [PK(b1)], [("slt", ft % 2)])
            tt("dve", aT[0][:, ft, :], sl, PSB[b3][:, 0:BS], ALU.mult, [("slt", ft % 2), PK(b3)], [("aT", 0, ft)])
        ak = [("aT", 0, ft) for ft in range(8)]
        for b in range(2):
            for h in range(2):
                bank = 6 + h
                for ft in range(8):
                    mm(PSB[bank], aT[0][:, ft, b * 128:(b + 1) * 128], Wb[wbuf][2][:, ft, h * 512:(h + 1) * 512],
                       ft == 0, ft == 7, ak + [("W", wbuf, 2)], [PK(bank)])
                cp("act" if h == 0 else "dve", ysb[0][:, b, h * 512:(h + 1) * 512], PSB[bank], [PK(bank)], [("ysb", 0, b)])
        dma("sp", y_d[s * BS:(s + 1) * BS, :].rearrange("(b p) d -> p b d", p=128), ysb[0], [("ysb", 0, 0), ("ysb", 0, 1)],
            [("y_d", s)], "ys%d" % wb)

    S.barrier()
    AR.off = mark_r
    NB = 4
    x1t = [A([1024], F32) for _ in range(NB)]
    ya = [A([1024], F32) for _ in range(NB)]
    yb = [A([1024], F32) for _ in range(NB)]
    ot = [A([1024], F32) for _ in range(NB)]
    ssq = A([16], F32)
    mset("dve", ssq, 0.0, [("ssqf", j) for j in range(16)])
    ydk = [("y_d", s_) for s_ in range(NSLOT)]

    def issue(j):
        b = j % NB
        dma("sp", x1t[b], x1_d[j * 128:(j + 1) * 128, :], [("x1_d", j)], [("x1t", b)], "x1l%d" % b)
        for k, dst in ((0, ya[b]), (1, yb[b])):
            S.dma("pool", (lambda j_, k_, d_: (lambda e: e.indirect_dma_start(
                out=d_, out_offset=None, in_=y_d[:, :],
                in_offset=bass.IndirectOffsetOnAxis(ap=dest_i[:, j_, k_:k_ + 1], axis=0))))(j, k, dst),
                ydk + [R], [("yab", b, k)], sem="g%d%d" % (b, k))

    for j in range(NB):
        issue(j)
    for j in range(16):
        b = j % NB
        act(ya[b], ya[b], AF.Copy, [("yab", b, 0), R], [("yab", b, 0)], scale=gate[:, j, 0:1])
        stt("dve", ya[b], yb[b], gate[:, j, 1:2], ya[b], ALU.mult, ALU.add, [("yab", b, 0), ("yab", b, 1), R], [("yab", b, 0)])
        tt("dve", ya[b], ya[b], g2_bc, ALU.mult, [("yab", b, 0), "g2_bc"], [("yab", b, 0)])
        tt("dve", ya[b], ya[b], x1t[b], ALU.add, [("yab", b, 0), ("x1t", b)], [("yab", b, 0)])
        act(ot[b], ya[b], AF.Square, [("yab", b, 0)], [("ot", b), ("ssqf", j)], accum=ssq[:, j:j + 1])
        act(ssq[:, j:j + 1], ssq[:, j:j + 1], AF.Sqrt, [("ssqf", j), "eps"], [("ssqf", j)], bias=eps_t, scale=1.0 / 1024.0)
        recip(ssq[:, j:j + 1], ssq[:, j:j + 1], [("ssqf", j)], [("ssqf", j)])
        stt("dve", ot[b], ya[b], ssq[:, j:j + 1], fg_bc, ALU.mult, ALU.mult, [("yab", b, 0), ("ssqf", j), "fg_bc"], [("ot", b)])
        dma("sp", out[j * 128:(j + 1) * 128, :], ot[b], [("ot", b)], [("out", j)], "os%d" % b)
        if j + NB < 16:
            issue(j + NB)
    S.final_wait("sp", [("out", j) for j in range(16)])


def _prep_core(c, I):
    b, hf = c // 2, c % 2
    x = I["x"][b]
    if hf == 0:
        seq = x
        ctx = I["ctx"][b]
    else:
        seq = x[::-1]
        ctx = I["ctx"][b][::-1]
    f = np.float32

    def fm(v, nt):
        return np.ascontiguousarray(np.asarray(v, f).reshape(nt, 128).T)
    vecs = np.zeros((128, NV), f)
    vecs[:, 0:8] = fm(I["norm1_g"][0], 8)
    vecs[:, 8:12] = fm(I["conv_b"][0], 4)
    vecs[:, 12:16] = fm(I["conv_ln_g"][0], 4)
    vecs[:, 16:20] = fm(I["conv_ln_b"][0], 4)
    vecs[:, 20:24] = fm(I["lru_conv_b"][0], 4)
    dirs = (0, 1) if hf == 0 else (1, 0)
    for di, d in enumerate(dirs):
        vecs[:, 24 + 12 * di:28 + 12 * di] = fm(I["lru_ba"][0, d], 4)
        vecs[:, 28 + 12 * di:32 + 12 * di] = fm(I["lru_bx"][0, d], 4)
        vecs[:, 32 + 12 * di:36 + 12 * di] = fm(I["lru_lam"][0, d], 4)
    w4 = np.asarray(I["lru_conv_w"][0], f)
    z = np.zeros((1, 512), f)
    w5 = np.concatenate([w4, z], 0) if hf == 0 else np.concatenate([z, w4[::-1]], 0)
    vecs[:, 48:68] = w5.T.reshape(4, 128, 5).transpose(1, 0, 2).reshape(128, 20)
    cd = np.asarray(I["conv_dw"][0], f)
    if hf == 1:
        cd = cd[::-1]
    cdw = np.ascontiguousarray(cd.T.reshape(4, 128, 31).transpose(1, 0, 2).reshape(128, 124))
    lbd = np.zeros((128, 16, 128), f)
    for di, d in enumerate(dirs):
        for g, nm in enumerate(("lru_wa", "lru_wx")):
            W = np.asarray(I[nm][0, d], f)
            for ct in range(4):
                for hh in range(2):
                    lbd[hh * 64:(hh + 1) * 64, (di * 2 + g) * 4 + ct, hh * 64:(hh + 1) * 64] = W[ct * 2 + hh]
    m = {
        "xT": np.ascontiguousarray(seq.T),
        "xtok": np.ascontiguousarray(seq[:2048]) if hf == 0 else np.ascontiguousarray(seq[:2048]),
        "ctxT": np.ascontiguousarray(ctx.T),
        "cvec": np.ascontiguousarray(np.stack([I["c"][b], I["c_ctx"]], 1).astype(f)),
        "vecs": vecs,
        "cdw": cdw,
        "lbd": lbd.reshape(128, 2048),
    }
    return m


def _prep_shared(I):
    f = np.float32
    ba = np.asarray(I["b_ada"][0], f).reshape(48, 128).T
    return {
        "w_ada": np.ascontiguousarray(I["w_ada"][0]),
        "b_ada": np.ascontiguousarray(np.repeat(ba[:, :, None], 2, 2).reshape(128, 96)),
        "n2g": np.ascontiguousarray(np.broadcast_to(np.asarray(I["norm2_g"][0], f)[None, :], (128, 1024))),
        "fg": np.ascontiguousarray(np.broadcast_to(np.asarray(I["final_g"], f)[None, :], (128, 1024))),
        "w_in": np.ascontiguousarray(I["w_in"][0]),
        "w_out": np.ascontiguousarray(I["w_out"][0]),
        "wr": np.ascontiguousarray(np.concatenate([I["router_wg"][0], I["router_we"][0].reshape(1024, 32)], 1).astype(f)),
        "br": np.ascontiguousarray(np.broadcast_to(
            np.concatenate([I["router_bg"][0], I["router_be"][0].reshape(32)])[None, :].astype(f), (128, 36))),
        "w1": np.asarray(I["w1"][0]).reshape(32768, 1024),
        "w3": np.asarray(I["w3"][0]).reshape(32768, 1024),
        "w2": np.asarray(I["w2"][0]).reshape(32768, 1024),
    }


def kernel(**inputs):
    I = {k: np.asarray(v) for k, v in inputs.items()}
    shared = _prep_shared(I)
    in_maps = []
    for c in range(8):
        m = dict(shared)
        m.update(_prep_core(c, I))
        in_maps.append(m)
    nc = build_nc()
    res = run_bass_kernel_spmd(nc, in_maps, core_ids=list(range(8)))
    outp = np.empty((4, 4096, 1024), np.float32)
    for c in range(8):
        b, hf = c // 2, c % 2
        o = np.asarray(res.results[c]["out"])
        if hf == 0:
            outp[b, 0:2048] = o
        else:
            outp[b, 2048:4096] = o[::-1]
    return outp
```

```python
import numpy as np
from contextlib import ExitStack
import concourse.bass as bass
import concourse.mybir as mybir
from concourse.bass_utils import run_bass_kernel_spmd

F32 = mybir.dt.float32
BF16 = mybir.dt.bfloat16
I32 = mybir.dt.int32
AF = mybir.ActivationFunctionType
ALU = mybir.AluOpType
AX = mybir.AxisListType

BS = 256
NSLOT = 48
NR = NSLOT * BS
NV = 68
NCV = 14


class Sched:
    ENG = ("pe", "act", "dve", "pool", "sp")

    def __init__(self, nc, stack):
        self.nc = nc
        self.stack = stack
        self.ops = {e: [] for e in self.ENG}
        self.cnt = {e: 0 for e in self.ENG}
        self.known = {e: {} for e in self.ENG}
        self.sem = {}
        self.reg = {}
        self.dstate = {}
        for e in self.ENG:
            self._sem("E:" + e)

    def _sem(self, key):
        if key not in self.sem:
            self.sem[key] = self.stack.enter_context(self.nc.semaphore(key.replace(":", "_")))
        return self.sem[key]

    def _deps(self, eng, reads, writes):
        deps = {}

        def add(k, v):
            if deps.get(k, 0) < v:
                deps[k] = v
        for k in reads:
            r = self.reg.get(k)
            if r and r[0]:
                add(*r[0])
        for k in writes:
            r = self.reg.get(k)
            if r:
                if r[0]:
                    add(*r[0])
                for sk, v in r[1].items():
                    add(sk, v)
        waits = []
        kn = self.known[eng]
        for sk, v in deps.items():
            if sk == "E:pe" and eng == "pe":
                continue
            if kn.get(sk, 0) < v:
                kn[sk] = v
                waits.append((sk, v))
        return waits

    def _mark(self, tok, reads, writes):
        for k in reads:
            r = self.reg.setdefault(k, [None, {}])
            if r[1].get(tok[0], 0) < tok[1]:
                r[1][tok[0]] = tok[1]
        for k in writes:
            self.reg[k] = [tok, {}]

    def op(self, eng, fn, reads=(), writes=(), inc=True):
        reads = list(reads)
        writes = list(writes)
        waits = self._deps(eng, reads, writes)
        sk = "E:" + eng
        if inc:
            self.cnt[eng] += 1
            tok = (sk, self.cnt[eng])
        else:
            tok = (sk, self.cnt[eng] + 1)
        self.ops[eng].append((waits, fn, (sk, 1) if inc else None))
        self._mark(tok, reads, writes)

    def dma(self, q, fn, reads=(), writes=(), sem="d", last=True):
        reads = list(reads)
        writes = list(writes)
        sk = "D:" + sem
        self._sem(sk)
        st = self.dstate.setdefault(sk, {"tot": 0, "open": []})
        waits = self._deps(q, reads, writes)
        if not st["open"] and st["tot"] > 0 and self.known[q].get(sk, 0) < st["tot"]:
            waits.append((sk, st["tot"]))
            self.known[q][sk] = st["tot"]
        st["tot"] += 16
        st["open"].append((reads, writes))
        self.ops[q].append((waits, fn, (sk, 16)))
        if last:
            tok = (sk, st["tot"])
            for r, w in st["open"]:
                self._mark(tok, r, w)
            st["open"] = []

    def barrier(self):
        for x in self.ENG:
            waits = []
            for e in self.ENG:
                if e == x:
                    continue
                sk = "E:" + e
                if self.cnt[e] > self.known[x].get(sk, 0):
                    self.known[x][sk] = self.cnt[e]
                    waits.append((sk, self.cnt[e]))
            for sk, st in self.dstate.items():
                assert not st["open"]
                if st["tot"] > self.known[x].get(sk, 0):
                    self.known[x][sk] = st["tot"]
                    waits.append((sk, st["tot"]))
            self.ops[x].append((waits, None, None))

    def final_wait(self, q, keys):
        waits = self._deps(q, list(keys), [])
        self.ops[q].append((waits, None, None))

    def emit(self):
        sems = self.sem
        with self.nc.Block() as block:
            def mk(name):
                def body(e):
                    for waits, fn, inc in self.ops[name]:
                        for sk, v in waits:
                            e.wait_ge(sems[sk], v)
                        if fn is None:
                            continue
                        ins = fn(e)
                        if inc is not None:
                            ins.then_inc(sems[inc[0]], inc[1])
                return body
            block.tensor(mk("pe"))
            block.scalar(mk("act"))
            block.vector(mk("dve"))
            block.gpsimd(mk("pool"))
            block.sync(mk("sp"))


class Arena:
    def __init__(self, base, nbytes):
        self.base = base
        self.total = nbytes
        self.off = 0

    def alloc(self, shape, dt):
        n = int(np.prod(shape))
        sz = n * (4 if dt in (F32, I32) else 2)
        szp = (sz + 31) // 32 * 32
        off = self.off
        self.off += szp
        assert self.off <= self.total, ("SBUF arena overflow", self.off, self.total)
        v = self.base[:, off // 4:(off + szp) // 4]
        if dt != F32:
            v = v.bitcast(dt)
        v = v[:, 0:n]
        if len(shape) == 2:
            v = v.rearrange("p (a b) -> p a b", a=shape[0])
        elif len(shape) == 3:
            v = v.rearrange("p (a b c) -> p a b c", a=shape[0], b=shape[1])
        return v


def build_nc(stage=9):
    nc = bass.Bass("TRN2", target_bir_lowering=False)

    def D(name, shape, dt, kind="ExternalInput"):
        return nc.dram_tensor(name, shape, dt, kind=kind).ap()
    xT = D("xT", [1024, 4096], F32)
    xtok = D("xtok", [2048, 1024], F32)
    ctxT = D("ctxT", [1024, 256], F32)
    cvec = D("cvec", [1024, 2], F32)
    w_ada = D("w_ada", [1024, 6144], F32)
    b_ada = D("b_ada", [128, 96], F32)
    vecs_d = D("vecs", [128, NV], F32)
    n2g_d = D("n2g", [128, 1024], F32)
    fg_d = D("fg", [128, 1024], F32)
    w_in = D("w_in", [1024, 2048], F32)
    w_out = D("w_out", [1024, 1024], F32)
    cdw_d = D("cdw", [128, 124], F32)
    lbd_d = D("lbd", [128, 2048], F32)
    wr_d = D("wr", [1024, 36], F32)
    br_d = D("br", [128, 36], F32)
    w1 = D("w1", [32768, 1024], F32)
    w3 = D("w3", [32768, 1024], F32)
    w2 = D("w2", [32768, 1024], F32)
    out = D("out", [2048, 1024], F32, "ExternalOutput")
    x1_d = D("x1_d", [2048, 1024], F32, "Internal")
    h2_d = D("h2_d", [2048, 1024], BF16, "Internal")
    xs_d = D("xs_d", [NR, 1024], BF16, "Internal")
    y_d = D("y_d", [NR, 1024], F32, "Internal")
    wbf = [D("wbf%d" % m, [NCV * 1024, 1024], BF16, "Internal") for m in range(3)]

    with ExitStack() as st:
        S = Sched(nc, st)
        ARB = 211968
        arena_t = st.enter_context(nc.sbuf_tensor("arena", [128, ARB // 4], F32))
        AR = Arena(arena_t[:, :], ARB)
        PSB = [st.enter_context(nc.psum_tensor("psb%d" % i, [128, 512], F32))[:, :] for i in range(8)]

        def PK(i):
            return ("ps", i)

        def mm(out_, lhsT, rhs, start, stop, reads, writes, inc=None):
            if inc is None:
                inc = stop
            S.op("pe", lambda e: e.matmul(out_, lhsT, rhs, start=start, stop=stop), reads, writes, inc=inc)

        def act(out_, in_, func, reads, writes, bias=None, scale=None, accum=None):
            kw = {}
            if bias is not None:
                kw["bias"] = bias
            if scale is not None:
                kw["scale"] = scale
            if accum is not None:
                kw["accum_out"] = accum
            S.op("act", lambda e: e.activation(out=out_, in_=in_, func=func, **kw), reads, writes)

        def tt(eng, out_, a, b, op, reads, writes):
            S.op(eng, lambda e: e.tensor_tensor(out=out_, in0=a, in1=b, op=op), reads, writes)

        def ts(eng, out_, a, s1, s2, op0, op1, reads, writes):
            if s2 is None:
                S.op(eng, lambda e: e.tensor_single_scalar(out=out_, in_=a, scalar=s1, op=op0), reads, writes)
            else:
                S.op(eng, lambda e: e.tensor_scalar(out=out_, in0=a, scalar1=s1, scalar2=s2, op0=op0, op1=op1), reads, writes)

        def stt(eng, out_, a, sc, b, op0, op1, reads, writes):
            S.op(eng, lambda e: e.scalar_tensor_tensor(out=out_, in0=a, scalar=sc, in1=b, op0=op0, op1=op1), reads, writes)

        def cp(eng, out_, in_, reads, writes):
            if eng == "act":
                S.op("act", lambda e: e.copy(out=out_, in_=in_), reads, writes)
            else:
                S.op(eng, lambda e: e.tensor_copy(out=out_, in_=in_), reads, writes)

        def recip(out_, in_, reads, writes):
            S.op("dve", lambda e: e.reciprocal(out=out_, in_=in_), reads, writes)

        def mset(eng, ap, val, writes):
            S.op(eng, lambda e: e.memset(ap, val), [], writes)

        def dma(q, out_, in_, reads, writes, sem, last=True):
            S.dma(q, lambda e: e.dma_start(out=out_, in_=in_), reads, writes, sem=sem, last=last)

        def red(eng, out_, in_, op, reads, writes):
            S.op(eng, lambda e: e.tensor_reduce(out=out_, in_=in_, axis=AX.X, op=op), reads, writes)

        wsrc = [w1, w3, w2]
        cv_list = [(m, e) for e in range(32 - NCV, 32) for m in range(3)]
        cv_pos = [0]

        def cv_next(n=1):
            for _ in range(n):
                if cv_pos[0] >= len(cv_list):
                    return
                m, e = cv_list[cv_pos[0]]
                i = cv_pos[0]
                cv_pos[0] += 1
                e0 = e - (32 - NCV)
                src = wsrc[m][e * 1024:(e + 1) * 1024, :]
                dst = wbf[m][e0 * 1024:(e0 + 1) * 1024, :]
                dma("pool", dst, src, [], [("wbf", m, e)], "cv%d" % (i % 4))

        iota_f = AR.alloc([128], F32)
        ident_f = AR.alloc([128], F32)
        ones_f = AR.alloc([128], F32)
        ident_b = AR.alloc([128], BF16)
        ones_b = AR.alloc([128], BF16)
        ones512_b = AR.alloc([128], BF16)
        U_b = AR.alloc([128], BF16)
        eps_t = AR.alloc([1], F32)
        one_t = AR.alloc([1], F32)
        vecs = AR.alloc([NV], F32)
        cdw = AR.alloc([124], F32)
        mod_fm = AR.alloc([96], F32)
        gs1 = AR.alloc([8, 2], F32)
        zbias = AR.alloc([32], F32)
        cs = AR.alloc([8], F32)
        hcs = AR.alloc([8], F32)
        hb = AR.alloc([24], F32)
        cv = AR.alloc([8, 2], F32)
        scT = AR.alloc([8, 2], BF16)
        scTf = AR.alloc([8, 2], F32)
        bada = AR.alloc([96], F32)
        carry = AR.alloc([8], F32)
        ldiag = AR.alloc([20, 128], BF16)
        lbd = AR.alloc([16, 128], BF16)
        lg_all = AR.alloc([16, 36], F32)
        PERSIST = AR.off
        MOEBASE = PERSIST

        V_N1G, V_CB, V_LNG, V_LNB, V_LCB = 0, 8, 12, 16, 20
        V_BA = (24, 36)
        V_BX = (28, 40)
        V_LAM = (32, 44)
        V_W5 = 48

        def vcol(i):
            return vecs[:, i:i + 1]

        def modv(v, t, r):
            c = (v * 8 + t) * 2 + r
            return mod_fm[:, c:c + 1]

        S.op("pool", lambda e: e.iota(iota_f, pattern=[[1, 128]], base=0, channel_multiplier=-1,
                                      allow_small_or_imprecise_dtypes=True), [], ["iota"])
        ts("dve", ident_f, iota_f, 0.0, None, ALU.is_equal, None, ["iota"], ["ident_f"])
        ts("dve", ident_b, iota_f, 0.0, None, ALU.is_equal, None, ["iota"], ["ident_b"])
        ts("dve", U_b, iota_f, 0.0, None, ALU.is_gt, None, ["iota"], ["U_b"])
        mset("pool", ones_f, 1.0, ["ones_f"])
        mset("pool", ones_b, 1.0, ["ones_b"])
        mset("pool", ones512_b, 1.0 / 512.0, ["ones512_b"])
        mset("pool", eps_t, 1e-6, ["eps"])
        mset("pool", one_t, 1.0, ["one"])
        dma("sp", vecs, vecs_d, [], ["vecs"], "c0")
        dma("sp", cdw, cdw_d, [], ["cdw"], "c1")
        dma("sp", cv, cvec.rearrange("(t p) r -> p t r", p=128), [], ["cv"], "c6")
        dma("sp", bada, b_ada, [], ["bada"], "c7")
        dma("pool", lbd.rearrange("p a b -> p (a b)"), lbd_d, [], ["lbd"], "c5")
        act(scT, cv, AF.Silu, ["cv"], ["scT"])
        act(scTf, cv, AF.Silu, ["cv"], ["scTf"])

        zl_buf = AR.alloc([4, 4100], BF16)
        zc_buf = AR.alloc([4, 260], BF16)
        convT = AR.alloc([4, 2048], BF16)
        gel = AR.alloc([4, 2048], BF16)
        markM = AR.off
        w_in_b = AR.alloc([8, 2048], BF16)
        sh1_b = AR.alloc([8, 2], BF16)
        cdg = [AR.alloc([31, 128], BF16) for _ in range(2)]
        xc = AR.alloc([8, 512], F32)
        sqhm = AR.alloc([8, 512], BF16)
        sqb = AR.alloc([8, 512], BF16)
        rtb = [AR.alloc([512], F32) for _ in range(2)]
        upad = AR.alloc([4, 8, 94], BF16)
        cc = AR.alloc([4, 512], F32)
        cbf = AR.alloc([4, 512], BF16)
        csq = AR.alloc([4, 512], BF16)
        ztile = AR.alloc([512], BF16)
        NT = 9
        tmp = [AR.alloc([512], F32) for _ in range(NT)]
        tctr = [0]
        print("M1 arena bytes", AR.off)

        def T():
            i = tctr[0] % NT
            tctr[0] += 1
            return tmp[i], ("tmp", i)

        psM = PSB[7]
        wab = [xc[:, :, 0:256].bitcast(BF16), sqhm]
        wabk = [["xc"], [("sqhm", t) for t in range(8)]]

        def ada_part(c_lo, c_hi, col0):
            for c in range(c_lo, c_hi):
                b = c % 2
                dma("pool", wab[b], w_ada.rearrange("(t p) n -> p t n", p=128)[:, :, c * 512:(c + 1) * 512],
                    [], wabk[b], "wab%d" % b)
                for ctl in range(4):
                    ct = c * 4 + ctl
                    for kt in range(8):
                        mm(psM[:, (ct - col0) * 2:(ct - col0) * 2 + 2], wab[b][:, kt, ctl * 128:(ctl + 1) * 128],
                           scT[:, kt, :], kt == 0, kt == 7, wabk[b] + ["scT"], [PK(7)])

        ada_part(0, 4, 0)
        tt("dve", mod_fm[:, 0:32], psM[:, 0:32], bada[:, 0:32], ALU.add, [PK(7), "bada"], ["mod_a"])
        sc1v = mod_fm[:, 16:32].rearrange("p (t r) -> p t r", r=2)
        sh1v = mod_fm[:, 0:16].rearrange("p (t r) -> p t r", r=2)
        for r in range(2):
            stt("dve", gs1[:, :, r], sc1v[:, :, r], 1.0, vecs[:, V_N1G:V_N1G + 8], ALU.add, ALU.mult,
                ["mod_a", "vecs"], ["gs1"])
        cp("dve", sh1_b, sh1v, ["mod_a"], ["sh1_b"])
        for d in range(2):
            lamv = vecs[:, V_LAM[d]:V_LAM[d] + 4]
            act(cs[:, d * 4:d * 4 + 4], lamv, AF.Exp, ["vecs"], [("cs", d)], scale=-1.0)
            act(cs[:, d * 4:d * 4 + 4], cs[:, d * 4:d * 4 + 4], AF.Ln, [("cs", d), "one"], [("cs", d)], bias=one_t)
            ts("dve", hcs[:, d * 4:d * 4 + 4], cs[:, d * 4:d * 4 + 4], -4.0, None, ALU.mult, None, [("cs", d)], [("hcs", d)])
        ts("dve", hb, vecs[:, 24:48], 0.5, None, ALU.mult, None, ["vecs"], ["hb"])
        for i in range(20):
            ts("dve", ldiag[:, i, :], ident_b, vcol(V_W5 + i), None, ALU.mult, None, ["ident_b", "vecs"], ["ldiag"])

        for kt in range(8):
            dma("pool", w_in_b[:, kt, :], w_in[kt * 128:(kt + 1) * 128, :], [], ["w_in_b"], "win", last=(kt == 7))
        mset("pool", zl_buf[:, :, 0:2], 0.0, ["zl_pad0"])
        mset("pool", zl_buf[:, :, 4098:4100], 0.0, ["zl_pad1"])
        mset("pool", zc_buf[:, :, 0:2], 0.0, ["zc_pad0"])
        mset("pool", zc_buf[:, :, 258:260], 0.0, ["zc_pad1"])
        mset("pool", upad.rearrange("p a b c -> p (a b c)"), 0.0, [("upad", c) for c in range(4)])
        for ct in range(16):
            for kt in range(8):
                mm(psM[:, 96 + ct * 2:98 + ct * 2], w_in_b[:, kt, ct * 128:(ct + 1) * 128], sh1_b[:, kt, :],
                   kt == 0, kt == 7, ["w_in_b", "sh1_b"], [PK(7)])
        cp("dve", zbias, psM[:, 96:128], [PK(7)], ["zbias"])

        def zb(ct, r):
            return zbias[:, ct * 2 + r:ct * 2 + r + 1]

        def norm_front(src, n0, N, par):
            dma("sp", xc[:, :, 0:N], src.rearrange("(t p) n -> p t n", p=128)[:, :, n0:n0 + N], [], ["xc"], "xc")
            act(sqb[:, :, 0:N], xc[:, :, 0:N], AF.Square, ["xc"], ["sqb"])
            for t in range(8):
                mm(PSB[6][:, 0:N], ones_b, sqb[:, t, 0:N], t == 0, t == 7, ["sqb", "ones_b"], [PK(6)])
            rt = rtb[par]
            act(rt[:, 0:N], PSB[6][:, 0:N], AF.Sqrt, [PK(6), "eps"], [("rtb", par)], bias=eps_t, scale=1.0 / 1024.0)
            recip(rt[:, 0:N], rt[:, 0:N], [("rtb", par)], [("rtb", par)])

        def norm_back(N, r, par):
            for t in range(8):
                stt("dve", sqhm[:, t, 0:N], xc[:, t, 0:N], gs1[:, t, r:r + 1], rtb[par][:, 0:N],
                    ALU.mult, ALU.mult, ["xc", "gs1", ("rtb", par)], [("sqhm", t)])

        def inproj(ct, N, bank):
            for kt in range(8):
                mm(PSB[bank][:, 0:N], w_in_b[:, kt, ct * 128:(ct + 1) * 128], sqhm[:, kt, 0:N], kt == 0, kt == 7,
                   [("sqhm", kt), "w_in_b"], [PK(bank)])
            return PSB[bank][:, 0:N], PK(bank)

        cx = {}
        for nm in ("ul", "rr", "ii", "aa"):
            cx[nm] = [tmp[i] for i in range(len(cx) * 2, len(cx) * 2 + 2)]

        def lru_steps(ulf, ulk, ulb, ubk, rr, ii, aa, key, d, c4s, N, inits, ikeys, reverse, b1, b2):
            for c4 in c4s:
                mm(PSB[b1][:, 0:N], lbd[:, (d * 2 + 0) * 4 + c4, :], ulb[c4][:, 0:N], True, True, [ubk(c4), "lbd"], [PK(b1)])
                mm(PSB[b2][:, 0:N], lbd[:, (d * 2 + 1) * 4 + c4, :], ulb[c4][:, 0:N], True, True, [ubk(c4), "lbd"], [PK(b2)])
                act(rr[c4][:, 0:N], PSB[b1][:, 0:N], AF.Tanh, [PK(b1), "hb"], [key("rr", c4)],
                    bias=hb[:, 12 * d + c4:12 * d + c4 + 1], scale=0.5)
                act(ii[c4][:, 0:N], PSB[b2][:, 0:N], AF.Tanh, [PK(b2), "hb"], [key("ii", c4)],
                    bias=hb[:, 12 * d + 4 + c4:12 * d + 5 + c4], scale=0.5)
            for c4 in c4s:
                hc = hcs[:, d * 4 + c4:d * 4 + c4 + 1]
                act(aa[c4][:, 0:N], rr[c4][:, 0:N], AF.Exp, [key("rr", c4), ("hcs", d)], [key("aa", c4)], bias=hc, scale=hc)
            for c4 in c4s:
                stt("dve", rr[c4][:, 0:N], aa[c4][:, 0:N], -1.0, aa[c4][:, 0:N], ALU.mult, ALU.mult, [key("aa", c4)], [key("rr", c4)])
                ts("dve", rr[c4][:, 0:N], rr[c4][:, 0:N], 1.0, 1e-12, ALU.add, ALU.max, [key("rr", c4)], [key("rr", c4)])
            for c4 in c4s:
                act(rr[c4][:, 0:N], rr[c4][:, 0:N], AF.Sqrt, [key("rr", c4)], [key("rr", c4)], scale=0.25)

        def lru_back(ulf, ulk, rr, ii, aa, key, c4s, N, inits, ikeys, reverse):
            for c4 in c4s:
                tt("pool", rr[c4][:, 0:N], rr[c4][:, 0:N], ulf[c4][:, 0:N], ALU.mult, [key("rr", c4), ulk(c4)], [key("rr", c4)])
            for c4 in c4s:
                stt("dve", ii[c4][:, 0:N], ii[c4][:, 0:N], 1.0, rr[c4][:, 0:N], ALU.add, ALU.mult,
                    [key("ii", c4), key("rr", c4)], [key("ii", c4)])
            for c4 in c4s:
                o_, a_, b_ = ii[c4][:, 0:N], aa[c4][:, 0:N], ii[c4][:, 0:N]
                if reverse:
                    o_, a_, b_ = o_[:, ::-1], a_[:, ::-1], b_[:, ::-1]
                S.op("dve", (lambda o__, a__, b__, i__: (lambda e: e.tensor_tensor_scan(
                    out=o__, data0=a__, data1=b__, initial=i__, op0=ALU.mult, op1=ALU.add)))(o_, a_, b_, inits[c4]),
                    [key("aa", c4), key("ii", c4)] + ikeys(c4), [key("ii", c4)])

        def lconv_all(buf, bkeys, c4s, m0, N, ulf, ulk, ulb, ubk, bank):
            for c4 in c4s:
                for tap in range(5):
                    mm(PSB[bank][:, 0:N], ldiag[:, c4 * 5 + tap, :], buf[:, c4, m0 + tap:m0 + tap + N], tap == 0, tap == 4,
                       bkeys(c4) + ["ldiag"], [PK(bank)])
                act(ulf[c4][:, 0:N], PSB[bank][:, 0:N], AF.Identity, [PK(bank), "vecs"], [ulk(c4)], bias=vcol(V_LCB + c4))
                cp("dve", ulb[c4][:, 0:N], ulf[c4][:, 0:N], [ulk(c4)], [ubk(c4)])

        norm_front(ctxT, 0, 256, 0)
        norm_back(256, 1, 0)
        for c4 in range(4):
            ps, pk = inproj(8 + c4, 256, c4 % 4)
            act(zc_buf[:, c4, 2:258], ps, AF.Identity, [pk, "zbias"], [("zc", c4)], bias=zb(8 + c4, 1))
        norm_front(xT, 4 * 512, 512, 1)
        c_ul = {0: tmp[0], 1: tmp[1]}
        c_ub = {0: tmp[2].bitcast(BF16), 1: tmp[2].bitcast(BF16)[:, 512:1024]}
        c_rr = {0: tmp[3], 1: tmp[4]}
        c_ii = {0: tmp[5], 1: tmp[6]}
        c_aa = {0: tmp[7], 1: tmp[8]}
        for pair in range(2):
            for d in range(2):
                loc = {0: 0, 1: 1}
                c4s = [pair * 2, pair * 2 + 1]
                ulf = {c4: c_ul[c4 % 2] for c4 in c4s}
                ulb = {c4: c_ub[c4 % 2] for c4 in c4s}
                rr = {c4: c_rr[c4 % 2] for c4 in c4s}
                ii = {c4: c_ii[c4 % 2] for c4 in c4s}
                aa = {c4: c_aa[c4 % 2] for c4 in c4s}
                key = lambda n, c4: ("tmp", {"rr": 3, "ii": 5, "aa": 7}[n] + c4 % 2)
                ulk = lambda c4: ("tmp", c4 % 2)
                ubk = lambda c4: ("tmp", 2)
                S.reg
                lconv_all(zc_buf, lambda c4: [("zc", c4), "zc_pad0", "zc_pad1"], c4s, 0, 256, ulf, ulk, ulb, ubk, 4)
                lru_steps(ulf, ulk, ulb, ubk, rr, ii, aa, key, d, c4s, 256, None, None, d == 1, 0, 1)
                lru_back(ulf, ulk, rr, ii, aa, key, c4s, 256, {c4: 0.0 for c4 in c4s}, lambda c4: [], d == 1)
                for c4 in c4s:
                    src = ii[c4][:, 255:256] if d == 0 else ii[c4][:, 0:1]
                    cp("pool", carry[:, d * 4 + c4:d * 4 + c4 + 1], src, [key("ii", c4)], [("carry", d, c4)])
        tctr[0] = 0

        norm_back(512, 0, 1)
        for k in range(4, 8):
            nxt = k + 1 if k < 7 else 0
            for c4 in range(4):
                ps, pk = inproj(8 + c4, 512, c4 % 4)
                act(zl_buf[:, c4, 2 + k * 512:2 + (k + 1) * 512], ps, AF.Identity, [pk, "zbias"], [("zl", c4, k)],
                    bias=zb(8 + c4, 0))
                if c4 == 1:
                    norm_front(xT, nxt * 512, 512, k % 2)
            norm_back(512, 0, k % 2)
            cv_next(1)

        for k in range(4):
            sl = slice(k * 512, (k + 1) * 512)
            for c4 in range(4):
                ps, pk = inproj(8 + c4, 512, c4 % 4)
                act(zl_buf[:, c4, 2 + k * 512:2 + (k + 1) * 512], ps, AF.Identity, [pk, "zbias"], [("zl", c4, k)],
                    bias=zb(8 + c4, 0))
            gz = []
            for c4 in range(4):
                ps, pk = inproj(12 + c4, 512, c4 % 4)
                zg, zgk = T()
                t1, t1k = T()
                act(zg, ps, AF.Identity, [pk, "zbias"], [zgk], bias=zb(12 + c4, 0))
                gz.append((zg, zgk, t1, t1k))
            for zg, zgk, t1, t1k in gz:
                tt("pool", t1, zg, zg, ALU.mult, [zgk], [t1k])
            for zg, zgk, t1, t1k in gz:
                ts("dve", t1, t1, 0.044715, 1.0, ALU.mult, ALU.add, [t1k], [t1k])
            for zg, zgk, t1, t1k in gz:
                tt("pool", t1, t1, zg, ALU.mult, [t1k, zgk], [t1k])
            for zg, zgk, t1, t1k in gz:
                act(t1, t1, AF.Sigmoid, [t1k], [t1k], scale=1.5957691216057308)
            for c4, (zg, zgk, t1, t1k) in enumerate(gz):
                tt("pool", gel[:, c4, sl], t1, zg, ALU.mult, [t1k, zgk], [("gel", c4, k)])
            if k < 3:
                norm_front(xT, (k + 1) * 512, 512, k % 2)

            def stageA(c4):
                bv, bg = 2 * (c4 % 2), 2 * (c4 % 2) + 1
                psv, pkv = inproj(c4, 512, bv)
                psg, pkg = inproj(4 + c4, 512, bg)
                val, valk = T()
                sg, sgk = T()
                act(val, psv, AF.Identity, [pkv, "zbias"], [valk], bias=zb(c4, 0))
                act(sg, psg, AF.Sigmoid, [pkg, "zbias"], [sgk], bias=zb(4 + c4, 0))
                tt("dve", upad[:, c4, :, 15:79], val.rearrange("p (a b) -> p a b", a=8),
                   sg.rearrange("p (a b) -> p a b", a=8), ALU.mult, [valk, sgk], [("upad", c4)])
                cb = c4 % 2
                for tap in range(31):
                    wcol = cdw[:, c4 * 31 + tap:c4 * 31 + tap + 1]
                    if tap % 3 == 2:
                        act(cdg[cb][:, tap, :], ident_b, AF.Copy, ["ident_b", "cdw"], [("cdg", cb, tap)], scale=wcol)
                    else:
                        ts("dve", cdg[cb][:, tap, :], ident_b, wcol, None, ALU.mult, None,
                           ["ident_b", "cdw"], [("cdg", cb, tap)])

            def stageB(c4):
                cb = c4 % 2
                bank = 4 + (c4 % 2)
                pcv = PSB[bank].rearrange("p (a b) -> p a b", a=8)
                for tap in range(31):
                    mm(pcv, cdg[cb][:, tap, :], upad[:, c4, :, tap:tap + 64], tap == 0, tap == 30,
                       [("cdg", cb, tap), ("upad", c4)], [PK(bank)])
                act(cc[:, c4, :], PSB[bank], AF.Identity, [PK(bank), "vecs"], [("cc", c4)], bias=vcol(V_CB + c4))
                act(csq[:, c4, :], cc[:, c4, :], AF.Square, [("cc", c4)], [("csq", c4)])
                cp("dve", cbf[:, c4, :], cc[:, c4, :], [("cc", c4)], [("cbf", c4)])

            cv_next(1)
            stageA(0)
            stageA(1)
            cv_next(1)
            stageB(0)
            stageA(2)
            cv_next(1)
            stageB(1)
            stageA(3)
            cv_next(2)
            stageB(2)
            stageB(3)
            for c4 in range(4):
                mm(PSB[6], ones512_b, cbf[:, c4, :], c4 == 0, c4 == 3, [("cbf", c4), "ones512_b"], [PK(6)])
            for c4 in range(4):
                mm(PSB[7], ones512_b, csq[:, c4, :], c4 == 0, c4 == 3, [("csq", c4), "ones512_b"], [PK(7)])
            m2, m2k = T()
            act(m2, PSB[6], AF.Square, [PK(6)], [m2k])
            tt("dve", m2, PSB[7], m2, ALU.subtract, [PK(7), m2k], [m2k])
            ts("dve", m2, m2, 0.0, None, ALU.max, None, [m2k], [m2k])
            act(m2, m2, AF.Sqrt, [m2k, "eps"], [m2k], bias=eps_t)
            recip(m2, m2, [m2k], [m2k])
            for c4 in range(4):
                dd, ddk = T()
                tt("dve", dd, cc[:, c4, :], PSB[6], ALU.subtract, [("cc", c4), PK(6)], [ddk])
                tt("pool", dd, dd, m2, ALU.mult, [ddk, m2k], [ddk])
                act(convT[:, c4, sl], dd, AF.Silu, [ddk, "vecs"], [("convT", c4, k)],
                    bias=vcol(V_LNB + c4), scale=vcol(V_LNG + c4))
            if k < 3:
                norm_back(512, 0, k % 2)

        S.barrier()
        AR.off = markM

        g1_bc = AR.alloc([1024], F32)
        gs2_bc = AR.alloc([1024], F32)
        sh2_bc = AR.alloc([1024], F32)
        wr_sb = AR.alloc([8, 36], F32)
        br_sb = AR.alloc([36], F32)
        dgt = [AR.alloc([128], F32) for _ in range(2)]
        hB_buf = AR.alloc([4, 2048], BF16)
        w_out_b = AR.alloc([8, 1024], BF16)
        mixl = AR.alloc([4, 512], BF16)
        xx_all = AR.alloc([4, 1024], F32)
        xt_t = [xx_all[:, 0, :], xx_all[:, 1, :]]
        x1_t = [xx_all[:, 2, :], xx_all[:, 3, :]]
        ztile2 = AR.alloc([512], BF16)
        h2_t = [AR.alloc([1024], F32) for _ in range(2)]
        h2T_tt = [AR.alloc([1024], F32) for _ in range(2)]
        h2b_t = [AR.alloc([1024], BF16) for _ in range(2)]
        ssq = AR.alloc([16], F32)
        m_ul = [AR.alloc([512], F32) for _ in range(4)]
        m_ub = [AR.alloc([512], BF16) for _ in range(4)]
        m_rr = [AR.alloc([512], F32) for _ in range(4)]
        m_ii = [AR.alloc([512], F32) for _ in range(4)]
        m_aa = [AR.alloc([512], F32) for _ in range(4)]
        wabf = xx_all.rearrange("p a b -> p (a b)").rearrange("p (t n) -> p t n", t=8)
        wabfk = [("xt", 0), ("xt", 1), ("x1", 0, 0), ("x1", 0, 1), ("x1", 1, 0), ("x1", 1, 1)]
        print("M2 arena bytes", AR.off)

        for kt in range(8):
            dma("pool", w_out_b[:, kt, :], w_out[kt * 128:(kt + 1) * 128, :], [], ["w_out_b"], "wout", last=(kt == 7))
        dma("sp", br_sb, br_d, [], ["br"], "c2")
        dma("sp", wr_sb, wr_d.rearrange("(t p) n -> p t n", p=128), [], ["wr"], "c3")
        dma("sp", gs2_bc, n2g_d, [], ["gs2_bc"], "c4")
        mset("pool", ssq, 0.0, [("ssq", j) for j in range(16)])
        def ada_dma(c):
            dma("sp", wabf, w_ada.rearrange("(t p) n -> p t n", p=128)[:, :, c * 512:(c + 1) * 512],
                [], wabfk, "wabf")

        def ada_mm(c, col0):
            for ctl in range(4):
                ct = c * 4 + ctl
                for kt in range(8):
                    mm(psM[:, (ct - col0) * 2:(ct - col0) * 2 + 2], wabf[:, kt, ctl * 128:(ctl + 1) * 128],
                       scTf[:, kt, :], kt == 0, kt == 7, wabfk + ["scTf"], [PK(7)])

        mset("dve", ztile2, 0.0, ["ztile2"])
        xs_flat = xs_d.rearrange("(p a) d -> p (a d)", p=128)
        NZ = NR * 1024 // 128 // 512
        zf_pos = [0]

        def zf_next(n):
            for _ in range(n):
                i = zf_pos[0]
                if i >= NZ:
                    return
                zf_pos[0] += 1
                dma("sp", xs_flat[:, i * 512:(i + 1) * 512], ztile2, ["ztile2"], ["xs_d"], "zf", last=(i == NZ - 1))

        def bcast_row(v, dst, key, post):
            for t in range(8):
                dg = dgt[t % 2]
                ts("dve", dg, ident_f, modv(v, t, 0), None, ALU.mult, None, ["ident_f", "mod_b"], [("dgt", t % 2)])
                bank = 5 + t // 4
                mm(PSB[bank][:, (t % 4) * 128:(t % 4 + 1) * 128], ones_f, dg, True, True,
                   [("dgt", t % 2), "ones_f"], [PK(bank)])
            for h in range(2):
                post(dst[:, h * 512:(h + 1) * 512], PSB[5 + h], [PK(5 + h)], [key])

        mkey = lambda n, c4: ("m" + n, c4)
        ulk = lambda c4: ("mul", c4)
        ubk = lambda c4: ("mub", c4)
        D4 = {c4: c4 for c4 in range(4)}
        ulf = {c4: m_ul[c4] for c4 in range(4)}
        ulb = {c4: m_ub[c4] for c4 in range(4)}
        rr = {c4: m_rr[c4] for c4 in range(4)}
        ii = {c4: m_ii[c4] for c4 in range(4)}
        aa = {c4: m_aa[c4] for c4 in range(4)}
        C4 = [0, 1, 2, 3]

        def zlkeys(k):
            return lambda c4: [("zl", c4, kk) for kk in range(max(0, k - 1), min(8, k + 2))] + ["zl_pad0", "zl_pad1"]

        for k in range(7, -1, -1):
            lconv_all(zl_buf, zlkeys(k), C4, k * 512, 512, ulf, ulk, ulb, ubk, 0)
            lru_steps(ulf, ulk, ulb, ubk, rr, ii, aa, mkey, 1, C4, 512, None, None, True, 1, 2)
            lru_back(ulf, ulk, rr, ii, aa, mkey, C4, 512, {c4: carry[:, 4 + c4:5 + c4] for c4 in C4},
                     lambda c4: [("carry", 1, c4)], True)
            for c4 in C4:
                cp("pool", carry[:, 4 + c4:5 + c4], ii[c4][:, 0:1], [mkey("ii", c4)], [("carry", 1, c4)])
                if k < 4:
                    cp("pool", hB_buf[:, c4, k * 512:(k + 1) * 512], ii[c4], [mkey("ii", c4)], [("hB", c4, k)])
            step = 7 - k
            if step >= 1:
                ada_mm(4 + step - 1, 16)
            ada_dma(4 + step)
            zf_next(24)
            cv_next(2)
        ada_mm(11, 16)
        tt("dve", mod_fm[:, 32:96], psM[:, 0:64], bada[:, 32:96], ALU.add, [PK(7), "bada"], ["mod_b"])
        bcast_row(2, g1_bc, "g1_bc", lambda o, p, r, w: cp("dve", o, p, r, w))
        bcast_row(3, sh2_bc, "sh2_bc", lambda o, p, r, w: cp("dve", o, p, r, w))
        bcast_row(4, gs2_bc, "gs2_bc",
                  lambda o, p, r, w: stt("dve", o, p, 1.0, o, ALU.add, ALU.mult, r + ["gs2_bc"], w))
        for kt in range(8):
            tt("pool", w_out_b[:, kt, :], w_out_b[:, kt, :], g1_bc, ALU.mult, ["w_out_b", "g1_bc"], ["w_out_b"])


        def tokX(k, tl):
            j = k * 4 + tl
            b = j % 2
            xt, x1, h2, h2b = xt_t[b], x1_t[b], h2_t[b], h2b_t[b]
            dma("sp", xt, xtok[j * 128:(j + 1) * 128, :], [], [("xt", b)], "xt%d" % b)
            for h in range(2):
                bank = 3 + h
                for kt in range(8):
                    if kt < 4:
                        lhs = convT[:, kt, j * 128:(j + 1) * 128]
                        rk_ = [("convT", kt, k)]
                    else:
                        lhs = mixl[:, kt - 4, tl * 128:(tl + 1) * 128]
                        rk_ = [("mixl", kt - 4)]
                    mm(PSB[bank], lhs, w_out_b[:, kt, h * 512:(h + 1) * 512], kt == 0, kt == 7,
                       rk_ + ["w_out_b"], [PK(bank)])
                hs = slice(h * 512, (h + 1) * 512)
                tt("dve", x1[:, hs], PSB[bank], xt[:, hs], ALU.add, [PK(bank), ("xt", b)], [("x1", b, h)])
            x1k = [("x1", b, 0), ("x1", b, 1)]
            dma("sp", x1_d[j * 128:(j + 1) * 128, :], x1, x1k, [("x1_d", j)], "x1s%d" % b)
            act(h2, x1, AF.Square, x1k, [("h2", b), ("ssq", j)], accum=ssq[:, j:j + 1])
            act(ssq[:, j:j + 1], ssq[:, j:j + 1], AF.Sqrt, [("ssq", j), "eps"], [("ssq", j)], bias=eps_t, scale=1.0 / 1024.0)
            recip(ssq[:, j:j + 1], ssq[:, j:j + 1], [("ssq", j)], [("ssq", j)])
            stt("dve", h2, x1, ssq[:, j:j + 1], gs2_bc, ALU.mult, ALU.mult, x1k + [("ssq", j), "gs2_bc"], [("h2", b)])
            tt("dve", h2, h2, sh2_bc, ALU.add, [("h2", b), "sh2_bc"], [("h2", b)])
            cp("act", h2b, h2, [("h2", b)], [("h2b", b)])
            dma("sp", h2_d[j * 128:(j + 1) * 128, :], h2b, [("h2b", b)], [("h2_d", j)], "h2s%d" % b)

        def tokYt(k, tl):
            j = k * 4 + tl
            b = j % 2
            h2 = h2_t[b]
            h2T_t = h2T_tt[b]
            for kt in range(8):
                bank = 5 + kt // 4
                S.op("pe", (lambda o_, i_: (lambda e: e.transpose(out=o_, in_=i_, identity=ident_f)))(
                    PSB[bank][:, (kt % 4) * 128:(kt % 4 + 1) * 128], h2[:, kt * 128:(kt + 1) * 128]),
                    [("h2", b), "ident_f"], [PK(bank)])
            cp("act", h2T_t[:, 0:512], PSB[5], [PK(5)], [("h2T", b, 0)])
            cp("dve", h2T_t[:, 512:1024], PSB[6], [PK(6)], [("h2T", b, 1)])

        def tokYl(k, tl):
            j = k * 4 + tl
            b = j % 2
            h2T_t = h2T_tt[b]
            for kt in range(8):
                mm(PSB[7][:, 0:36], h2T_t[:, kt * 128:(kt + 1) * 128], wr_sb[:, kt, :], kt == 0, kt == 7,
                   [("h2T", b, kt // 4), "wr"], [PK(7)])
            tt("dve", lg_all[:, j, :], PSB[7][:, 0:36], br_sb, ALU.add, [PK(7), "br"], [("lg", j)])

        def token_stage(k):
            tokX(k, 0)
            tokX(k, 1)
            tokYt(k, 0)
            tokX(k, 2)
            tokYt(k, 1)
            tokYl(k, 0)
            tokX(k, 3)
            tokYt(k, 2)
            tokYl(k, 1)
            tokYt(k, 3)
            tokYl(k, 2)
            tokYl(k, 3)

        for k in range(4):
            lconv_all(zl_buf, zlkeys(k), C4, k * 512, 512, ulf, ulk, ulb, ubk, 0)
            lru_steps(ulf, ulk, ulb, ubk, rr, ii, aa, mkey, 0, C4, 512, None, None, False, 1, 2)
            cv_next(3)
            if k > 0:
                token_stage(k - 1)
            lru_back(ulf, ulk, rr, ii, aa, mkey, C4, 512, {c4: carry[:, c4:c4 + 1] for c4 in C4},
                     lambda c4: [("carry", 0, c4)], False)
            for c4 in C4:
                cp("pool", carry[:, c4:c4 + 1], ii[c4][:, 511:512], [mkey("ii", c4)], [("carry", 0, c4)])
                tt("pool", ii[c4], ii[c4], hB_buf[:, c4, k * 512:(k + 1) * 512], ALU.add, [mkey("ii", c4), ("hB", c4, k)],
                   [mkey("ii", c4)])
                tt("pool", mixl[:, c4, :], ii[c4], gel[:, c4, k * 512:(k + 1) * 512], ALU.mult,
                   [mkey("ii", c4), ("gel", c4, k)], [("mixl", c4)])
        token_stage(3)
        cv_next(len(cv_list))

        S.barrier()
        AR.off = MOEBASE
        if stage == 1:
            for j in range(16):
                dma("sp", out[j * 128:(j + 1) * 128, :], x1_d[j * 128:(j + 1) * 128, :], [("x1_d", j)], [("out", j)], "os0")
            S.final_wait("sp", [("out", j) for j in range(16)])
        else:
            build_moe(nc, S, AR, PSB, locals())
        S.emit()
    return nc


def build_moe(nc, S, AR, PSB, L):
    (mm, act, tt, ts, stt, cp, recip, mset, dma, red) = (L[k] for k in
                                                         ("mm", "act", "tt", "ts", "stt", "cp", "recip", "mset", "dma", "red"))
    lg_all, ones_b, U_b, ident_b, ident_f, ones_f, eps_t, mod_fm = (L[k] for k in (
        "lg_all", "ones_b", "U_b", "ident_b", "ident_f", "ones_f", "eps_t", "mod_fm"))
    x1_d, h2_d, xs_d, y_d, out, fg_d = (L[k] for k in ("x1_d", "h2_d", "xs_d", "y_d", "out", "fg_d"))
    wbf = L["wbf"]
    w1, w3, w2 = L["w1"], L["w3"], L["w2"]
    modv = L["modv"]

    def PK(i):
        return ("ps", i)

    A = AR.alloc
    lgk = [("lg", j) for j in range(16)]
    gate = A([16, 2], F32)
    dest_i = A([16, 2], I32)
    idx_i = A([NSLOT], I32)
    idxB_i = A([NSLOT], I32)
    g2_bc = A([1024], F32)
    fg_bc = A([1024], F32)
    mark_r = AR.off
    gmax = A([16], F32)
    oh = A([16, 4], F32)
    d4 = A([16, 4], F32)
    pg = A([16], F32)
    sel = A([16, 32], F32)
    sel2 = A([16, 32], F32)
    mask1 = A([16, 32], F32)
    mask2 = A([16, 32], F32)
    m1 = A([16], F32)
    m2 = A([16], F32)
    Mb = A([512], BF16)
    tcnt = A([16, 32], F32)
    rank = A([16, 32], F32)
    off = A([16, 32], F32)
    cnt = A([32], F32)
    cnt_i = A([32], I32)
    padded = A([32], F32)
    pend = A([32], F32)
    pstart = A([32], F32)
    ones32 = A([32], F32)
    pos = A([16, 32], F32)
    pm = A([16, 32], F32)
    dest_f = A([16, 2], F32)
    sbs = A([NSLOT], F32)
    cmp = A([NSLOT, 32], F32)
    blk = A([NSLOT], F32)
    base8 = A([1], F32)
    idx_f = A([NSLOT], F32)
    idxA_f = A([NSLOT], F32)
    same = A([NSLOT], F32)
    pmask = A([1], F32)
    dgt = [A([128], F32) for _ in range(2)]

    R = "rt"
    lgv = lg_all[:, :, 0:4]
    lev = lg_all[:, :, 4:36]
    red("dve", gmax, lgv, ALU.max, lgk, [R])
    tt("dve", oh, lgv, gmax.unsqueeze(2).to_broadcast([128, 16, 4]), ALU.is_equal, lgk + [R], [R])
    tt("dve", d4, lgv, gmax.unsqueeze(2).to_broadcast([128, 16, 4]), ALU.subtract, lgk + [R], [R])
    act(d4, d4, AF.Exp, [R], [R])
    red("dve", pg, d4, ALU.add, [R], [R])
    recip(pg, pg, [R], [R])
    ts("dve", oh, oh, 1e30, -1e30, ALU.mult, ALU.add, [R], [R])
    tt("dve", sel.rearrange("p j (g e) -> p j g e", g=4), lev.rearrange("p j (g e) -> p j g e", g=4),
       oh.unsqueeze(3).to_broadcast([128, 16, 4, 8]), ALU.add, lgk + [R], [R])
    red("dve", m1, sel, ALU.max, [R], [R])
    tt("dve", mask1, sel, m1.unsqueeze(2).to_broadcast([128, 16, 32]), ALU.is_equal, [R], [R])
    stt("dve", sel2, mask1, -1e30, sel, ALU.mult, ALU.add, [R], [R])
    red("dve", m2, sel2, ALU.max, [R], [R])
    tt("dve", mask2, sel2, m2.unsqueeze(2).to_broadcast([128, 16, 32]), ALU.is_equal, [R], [R])
    tt("dve", m2, m2, m1, ALU.subtract, [R], [R])
    act(m2, m2, AF.Exp, [R], [R])
    ts("dve", m2, m2, 1.0, None, ALU.add, None, [R], [R])
    recip(m2, m2, [R], [R])
    tt("dve", gate[:, :, 0], pg, m2, ALU.mult, [R], [R])
    tt("dve", gate[:, :, 1], pg, gate[:, :, 0], ALU.subtract, [R], [R])
    tt("dve", Mb.rearrange("p (j e) -> p j e", j=16), mask1, mask2, ALU.add, [R], [R])
    mm(PSB[0], ones_b, Mb, True, True, [R, "ones_b"], [PK(0)])
    mm(PSB[1], U_b, Mb, True, True, [R, "U_b"], [PK(1)])
    cp("dve", tcnt.rearrange("p j e -> p (j e)"), PSB[0], [PK(0)], [R])
    cp("dve", rank.rearrange("p j e -> p (j e)"), PSB[1], [PK(1)], [R])
    mset("dve", off[:, 0, :], 0.0, [R])
    for j in range(1, 16):
        tt("dve", off[:, j, :], off[:, j - 1, :], tcnt[:, j - 1, :], ALU.add, [R], [R])
    tt("dve", cnt, off[:, 15, :], tcnt[:, 15, :], ALU.add, [R], [R])
    cp("dve", cnt_i, cnt, [R], [R])
    ts("dve", cnt_i, cnt_i, BS - 1, None, ALU.add, None, [R], [R])
    ts("dve", cnt_i, cnt_i, 8, None, ALU.arith_shift_right, None, [R], [R])
    ts("dve", cnt_i, cnt_i, 8, None, ALU.logical_shift_left, None, [R], [R])
    cp("dve", padded, cnt_i, [R], [R])
    mset("dve", ones32, 1.0, [R])
    S.op("dve", lambda e: e.tensor_tensor_scan(out=pend, data0=ones32, data1=padded, initial=0.0,
                                               op0=ALU.mult, op1=ALU.add), [R], [R])
    tt("dve", pstart, pend, padded, ALU.subtract, [R], [R])
    tt("dve", pos, rank, off, ALU.add, [R], [R])
    tt("dve", pos, pos, pstart.unsqueeze(1).to_broadcast([128, 16, 32]), ALU.add, [R], [R])
    tt("dve", pm, pos, mask1, ALU.mult, [R], [R])
    red("dve", dest_f[:, :, 0], pm, ALU.add, [R], [R])
    tt("dve", pm, pos, mask2, ALU.mult, [R], [R])
    red("dve", dest_f[:, :, 1], pm, ALU.add, [R], [R])
    cp("dve", dest_i, dest_f, [R], [R])
    S.op("pool", lambda e: e.iota(sbs, pattern=[[BS, NSLOT]], base=0, channel_multiplier=0,
                                  allow_small_or_imprecise_dtypes=True), [], ["sbs"])
    S.op("pool", lambda e: e.iota(base8, pattern=[[0, 1]], base=0, channel_multiplier=1,
                                  allow_small_or_imprecise_dtypes=True), [], ["base8"])
    tt("dve", cmp, pend.unsqueeze(1).to_broadcast([128, NSLOT, 32]), sbs.unsqueeze(2).to_broadcast([128, NSLOT, 32]),
       ALU.is_le, [R, "sbs"], [R])
    red("dve", blk, cmp, ALU.add, [R], [R])
    ts("dve", blk, blk, 31.0, 128.0, ALU.min, ALU.mult, [R], [R])
    ts("dve", idx_f, blk, base8[:, 0:1], None, ALU.add, None, [R, "base8"], [R])
    S.op("pool", lambda e: e.iota(pmask, pattern=[[0, 1]], base=0, channel_multiplier=1,
                                  allow_small_or_imprecise_dtypes=True), [], ["pmask"])
    ts("dve", pmask, pmask, 0.5, None, ALU.is_gt, None, ["pmask"], ["pmask"])
    mset("dve", same, 0.0, [R])
    H = NSLOT // 2
    tt("dve", same[:, 2:H], blk[:, 2:H], blk[:, 0:H - 2], ALU.is_equal, [R], [R])
    tt("dve", same[:, H:NSLOT - 1], blk[:, H:NSLOT - 1], blk[:, H + 1:NSLOT], ALU.is_equal, [R], [R])
    ts("dve", same, same, pmask[:, 0:1], 6.0e4, ALU.mult, ALU.mult, [R, "pmask"], [R])
    tt("dve", idx_f, idx_f, same, ALU.add, [R], [R])
    cp("dve", idx_i, idx_f, [R], [R])
    OFF = float((32 - NCV) * 128)
    ts("dve", same, blk, OFF - 0.5, 6.0e4, ALU.is_lt, ALU.mult, [R], [R])
    stt("dve", idxA_f, idx_f, -OFF, same, ALU.add, ALU.add, [R], [R])
    cp("dve", idx_i, idxA_f, [R], [R])
    ts("dve", same, same, -1.0, 6.0e4, ALU.mult, ALU.add, [R], [R])
    tt("dve", idx_f, idx_f, same, ALU.add, [R], [R])
    cp("dve", idxB_i, idx_f, [R], [R])

    dma("sp", fg_bc, fg_d, [], ["fg_bc"], "c0")
    for t in range(8):
        dg = dgt[t % 2]
        ts("dve", dg, ident_f, modv(5, t, 0), None, ALU.mult, None, ["ident_f", "mod_fm"], [("dgt", t % 2)])
        bank = 5 + t // 4
        mm(PSB[bank][:, (t % 4) * 128:(t % 4 + 1) * 128], ones_f, dg, True, True, [("dgt", t % 2), "ones_f"], [PK(bank)])
    for h in range(2):
        cp("dve", g2_bc[:, h * 512:(h + 1) * 512], PSB[5 + h], [PK(5 + h)], ["g2_bc"])

    S.barrier()
    AR.off = mark_r
    hall = A([16, 1024], BF16)
    dma("sp", hall, h2_d.rearrange("(j p) d -> p j d", p=128), [("h2_d", j) for j in range(16)], ["hall"], "hall")
    for j in range(16):
        for k in range(2):
            S.dma("pool", (lambda j_, k_: (lambda e: e.indirect_dma_start(
                out=xs_d[:, :], out_offset=bass.IndirectOffsetOnAxis(ap=dest_i[:, j_, k_:k_ + 1], axis=0),
                in_=hall[:, j_, :], in_offset=None)))(j, k),
                ["hall", R], ["xs_d"], sem="scat", last=(j == 15 and k == 1))

    S.barrier()
    AR.off = mark_r
    Wb = [[A([8, 1024], BF16) for _ in range(3)] for _ in range(3)]
    xtm = [A([2, 1024], BF16) for _ in range(2)]
    XeT = [A([8, 256], BF16) for _ in range(2)]
    aT = [A([8, 256], BF16) for _ in range(1)]
    ysb = [A([2, 1024], F32) for _ in range(1)]
    print("MoE arena bytes", AR.off)
    slt = [A([256], F32) for _ in range(2)]
    wd = [w1, w3, w2]
    bc_cache = {}

    def bc_reg(e, val):
        if val not in bc_cache:
            bc_cache[val] = e.to_reg(val)
        return bc_cache[val]

    order = []
    for i in range(NSLOT // 2):
        order.append((i, i % 2))
        order.append((NSLOT - 1 - i, 2))
    for oi, (s, wbuf) in enumerate(order):
        wb = oi % 2
        for m in range(3):
            S.dma("pool", (lambda m_, s_, wb_: (lambda e: e.indirect_dma_start(
                out=Wb[wb_][m_].rearrange("p a b -> p (a b)"), out_offset=None,
                in_=wbf[m_].rearrange("(r k) n -> r (k n)", k=8),
                in_offset=bass.IndirectOffsetOnAxis(ap=idx_i[:, s_:s_ + 1], axis=0),
                bounds_check=bc_reg(e, NCV * 128 - 1), oob_is_err=False)))(m, s, wbuf),
                [R] + [("wbf", m, e_) for e_ in range(32 - NCV, 32)], [("W", wbuf, m)], sem="w%d%d" % (wbuf, m), last=False)
            S.dma("pool", (lambda m_, s_, wb_: (lambda e: e.indirect_dma_start(
                out=Wb[wb_][m_].rearrange("p a b -> p (a b)"), out_offset=None,
                in_=wd[m_].rearrange("(r k) n -> r (k n)", k=8),
                in_offset=bass.IndirectOffsetOnAxis(ap=idxB_i[:, s_:s_ + 1], axis=0),
                bounds_check=bc_reg(e, 4095), oob_is_err=False)))(m, s, wbuf),
                [R], [("W", wbuf, m)], sem="w%d%d" % (wbuf, m))
        if oi == 0:
            dma("sp", xtm[0], xs_d[s * BS:(s + 1) * BS, :].rearrange("(b p) d -> p b d", p=128), ["xs_d"], [("xtm", 0)], "xtm0")
        if oi + 1 < NSLOT:
            nb = (oi + 1) % 2
            s2 = order[oi + 1][0]
            dma("sp", xtm[nb], xs_d[s2 * BS:(s2 + 1) * BS, :].rearrange("(b p) d -> p b d", p=128), ["xs_d"],
                [("xtm", nb)], "xtm%d" % nb)
        for b in range(2):
            bank = b
            pst = PSB[bank].bitcast(BF16)
            for kt in range(8):
                S.op("pe", (lambda o_, i_: (lambda e: e.transpose(out=o_, in_=i_, identity=ident_b)))(
                    pst[:, kt * 128:(kt + 1) * 128], xtm[wb][:, b, kt:1024:8]),
                    [("xtm", wb), "ident_b"], [PK(bank)])
            cp("act" if b == 0 else "dve", XeT[wb][:, :, b * 128:(b + 1) * 128],
               pst.rearrange("p (k r) -> p k r", k=8), [PK(bank)], [("XeT", wb, b)])
        xk = [("XeT", wb, 0), ("XeT", wb, 1)]
        for ft in range(8):
            b1 = 2 + (ft % 2)
            b3 = 4 + (ft % 2)
            for kt in range(8):
                mm(PSB[b1][:, 0:BS], Wb[wbuf][0][:, kt, ft:1024:8], XeT[wb][:, kt, :], kt == 0, kt == 7,
                   xk + [("W", wbuf, 0)], [PK(b1)])
            for kt in range(8):
                mm(PSB[b3][:, 0:BS], Wb[wbuf][1][:, kt, ft:1024:8], XeT[wb][:, kt, :], kt == 0, kt == 7,
                   xk + [("W", wbuf, 1)], [PK(b3)])
            sl = slt[ft % 2]
            act(sl, PSB[b1][:, 0:BS], AF.Silu, [PK(b1)], [("slt", ft % 2)])
            tt("dve", aT[0][:, ft, :], sl, PSB[b3][:, 0:BS], ALU.mult, [("slt", ft % 2), PK(b3)], [("aT", 0, ft)])
        ak = [("aT", 0, ft) for ft in range(8)]
        for b in range(2):
            for h in range(2):
                bank = 6 + h
                for ft in range(8):
                    mm(PSB[bank], aT[0][:, ft, b * 128:(b + 1) * 128], Wb[wbuf][2][:, ft, h * 512:(h + 1) * 512],
                       ft == 0, ft == 7, ak + [("W", wbuf, 2)], [PK(bank)])
                cp("act" if h == 0 else "dve", ysb[0][:, b, h * 512:(h + 1) * 512], PSB[bank], [PK(bank)], [("ysb", 0, b)])
        dma("sp", y_d[s * BS:(s + 1) * BS, :].rearrange("(b p) d -> p b d", p=128), ysb[0], [("ysb", 0, 0), ("ysb", 0, 1)],
            [("y_d", s)], "ys%d" % wb)

    S.barrier()
    AR.off = mark_r
    NB = 4
    x1t = [A([1024], F32) for _ in range(NB)]
    ya = [A([1024], F32) for _ in range(NB)]
    yb = [A([1024], F32) for _ in range(NB)]
    ot = [A([1024], F32) for _ in range(NB)]
    ssq = A([16], F32)
    mset("dve", ssq, 0.0, [("ssqf", j) for j in range(16)])
    ydk = [("y_d", s_) for s_ in range(NSLOT)]

    def issue(j):
        b = j % NB
        dma("sp", x1t[b], x1_d[j * 128:(j + 1) * 128, :], [("x1_d", j)], [("x1t", b)], "x1l%d" % b)
        for k, dst in ((0, ya[b]), (1, yb[b])):
            S.dma("pool", (lambda j_, k_, d_: (lambda e: e.indirect_dma_start(
                out=d_, out_offset=None, in_=y_d[:, :],
                in_offset=bass.IndirectOffsetOnAxis(ap=dest_i[:, j_, k_:k_ + 1], axis=0))))(j, k, dst),
                ydk + [R], [("yab", b, k)], sem="g%d%d" % (b, k))

    for j in range(NB):
        issue(j)
    for j in range(16):
        b = j % NB
        act(ya[b], ya[b], AF.Copy, [("yab", b, 0), R], [("yab", b, 0)], scale=gate[:, j, 0:1])
        stt("dve", ya[b], yb[b], gate[:, j, 1:2], ya[b], ALU.mult, ALU.add, [("yab", b, 0), ("yab", b, 1), R], [("yab", b, 0)])
        tt("dve", ya[b], ya[b], g2_bc, ALU.mult, [("yab", b, 0), "g2_bc"], [("yab", b, 0)])
        tt("dve", ya[b], ya[b], x1t[b], ALU.add, [("yab", b, 0), ("x1t", b)], [("yab", b, 0)])
        act(ot[b], ya[b], AF.Square, [("yab", b, 0)], [("ot", b), ("ssqf", j)], accum=ssq[:, j:j + 1])
        act(ssq[:, j:j + 1], ssq[:, j:j + 1], AF.Sqrt, [("ssqf", j), "eps"], [("ssqf", j)], bias=eps_t, scale=1.0 / 1024.0)
        recip(ssq[:, j:j + 1], ssq[:, j:j + 1], [("ssqf", j)], [("ssqf", j)])
        stt("dve", ot[b], ya[b], ssq[:, j:j + 1], fg_bc, ALU.mult, ALU.mult, [("yab", b, 0), ("ssqf", j), "fg_bc"], [("ot", b)])
        dma("sp", out[j * 128:(j + 1) * 128, :], ot[b], [("ot", b)], [("out", j)], "os%d" % b)
        if j + NB < 16:
            issue(j + NB)
    S.final_wait("sp", [("out", j) for j in range(16)])


def _prep_core(c, I):
    b, hf = c // 2, c % 2
    x = I["x"][b]
    if hf == 0:
        seq = x
        ctx = I["ctx"][b]
    else:
        seq = x[::-1]
        ctx = I["ctx"][b][::-1]
    f = np.float32

    def fm(v, nt):
        return np.ascontiguousarray(np.asarray(v, f).reshape(nt, 128).T)
    vecs = np.zeros((128, NV), f)
    vecs[:, 0:8] = fm(I["norm1_g"][0], 8)
    vecs[:, 8:12] = fm(I["conv_b"][0], 4)
    vecs[:, 12:16] = fm(I["conv_ln_g"][0], 4)
    vecs[:, 16:20] = fm(I["conv_ln_b"][0], 4)
    vecs[:, 20:24] = fm(I["lru_conv_b"][0], 4)
    dirs = (0, 1) if hf == 0 else (1, 0)
    for di, d in enumerate(dirs):
        vecs[:, 24 + 12 * di:28 + 12 * di] = fm(I["lru_ba"][0, d], 4)
        vecs[:, 28 + 12 * di:32 + 12 * di] = fm(I["lru_bx"][0, d], 4)
        vecs[:, 32 + 12 * di:36 + 12 * di] = fm(I["lru_lam"][0, d], 4)
    w4 = np.asarray(I["lru_conv_w"][0], f)
    z = np.zeros((1, 512), f)
    w5 = np.concatenate([w4, z], 0) if hf == 0 else np.concatenate([z, w4[::-1]], 0)
    vecs[:, 48:68] = w5.T.reshape(4, 128, 5).transpose(1, 0, 2).reshape(128, 20)
    cd = np.asarray(I["conv_dw"][0], f)
    if hf == 1:
        cd = cd[::-1]
    cdw = np.ascontiguousarray(cd.T.reshape(4, 128, 31).transpose(1, 0, 2).reshape(128, 124))
    lbd = np.zeros((128, 16, 128), f)
    for di, d in enumerate(dirs):
        for g, nm in enumerate(("lru_wa", "lru_wx")):
            W = np.asarray(I[nm][0, d], f)
            for ct in range(4):
                for hh in range(2):
                    lbd[hh * 64:(hh + 1) * 64, (di * 2 + g) * 4 + ct, hh * 64:(hh + 1) * 64] = W[ct * 2 + hh]
    m = {
        "xT": np.ascontiguousarray(seq.T),
        "xtok": np.ascontiguousarray(seq[:2048]) if hf == 0 else np.ascontiguousarray(seq[:2048]),
        "ctxT": np.ascontiguousarray(ctx.T),
        "cvec": np.ascontiguousarray(np.stack([I["c"][b], I["c_ctx"]], 1).astype(f)),
        "vecs": vecs,
        "cdw": cdw,
        "lbd": lbd.reshape(128, 2048),
    }
    return m


def _prep_shared(I):
    f = np.float32
    ba = np.asarray(I["b_ada"][0], f).reshape(48, 128).T
    return {
        "w_ada": np.ascontiguousarray(I["w_ada"][0]),
        "b_ada": np.ascontiguousarray(np.repeat(ba[:, :, None], 2, 2).reshape(128, 96)),
        "n2g": np.ascontiguousarray(np.broadcast_to(np.asarray(I["norm2_g"][0], f)[None, :], (128, 1024))),
        "fg": np.ascontiguousarray(np.broadcast_to(np.asarray(I["final_g"], f)[None, :], (128, 1024))),
        "w_in": np.ascontiguousarray(I["w_in"][0]),
        "w_out": np.ascontiguousarray(I["w_out"][0]),
        "wr": np.ascontiguousarray(np.concatenate([I["router_wg"][0], I["router_we"][0].reshape(1024, 32)], 1).astype(f)),
        "br": np.ascontiguousarray(np.broadcast_to(
            np.concatenate([I["router_bg"][0], I["router_be"][0].reshape(32)])[None, :].astype(f), (128, 36))),
        "w1": np.asarray(I["w1"][0]).reshape(32768, 1024),
        "w3": np.asarray(I["w3"][0]).reshape(32768, 1024),
        "w2": np.asarray(I["w2"][0]).reshape(32768, 1024),
    }


def kernel(**inputs):
    I = {k: np.asarray(v) for k, v in inputs.items()}
    shared = _prep_shared(I)
    in_maps = []
    for c in range(8):
        m = dict(shared)
        m.update(_prep_core(c, I))
        in_maps.append(m)
    nc = build_nc()
    res = run_bass_kernel_spmd(nc, in_maps, core_ids=list(range(8)))
    outp = np.empty((4, 4096, 1024), np.float32)
    for c in range(8):
        b, hf = c // 2, c % 2
        o = np.asarray(res.results[c]["out"])
        if hf == 0:
            outp[b, 0:2048] = o
        else:
            outp[b, 2048:4096] = o[::-1]
    return outp
```
